# Optimizing a Trainium2 kernel written in Bass

```python
import jax, jax.numpy as jnp
from jax import lax
import numpy as np


D_MODEL = 1024
BATCH = 4
SEQ = 4096
DEPTH = 2

D_MIX = D_MODEL
SC_W = D_MIX // 4
SC_K = 3
CF_W = D_MIX // 4
CF_K = 31
GLA_DV = D_MIX // 2
GLA_DK = GLA_DV // 2
GLA_H = 4
GLA_HDK = GLA_DK // GLA_H
GLA_HDV = GLA_DV // GLA_H
GLA_RANK = 16
GLA_TAU = 16.0
GLA_CHUNK = 64
D_FF = 2816
N_EXPERTS = 8
TOP_K = 2
N_DENSE = (DEPTH + 1) // 2
N_MOE = DEPTH // 2
EPS = 1e-6
IN_SIZES = (SC_W, SC_W, SC_W,
            CF_W, CF_W,
            GLA_DK, GLA_DK, GLA_DV,
            GLA_RANK, GLA_DV)
D_IN = sum(IN_SIZES)

kernel_name = "hybrid_conv_conformer_gla_moe_block"


def _rmsnorm(x, g):
    xf = x.astype(jnp.float32)
    y = xf * lax.rsqrt(jnp.mean(xf * xf, axis=-1, keepdims=True) + EPS)
    return (y * g.astype(jnp.float32)).astype(x.dtype)


def _layernorm(x, g, b):
    xf = x.astype(jnp.float32)
    mu = jnp.mean(xf, axis=-1, keepdims=True)
    var = jnp.mean(jnp.square(xf - mu), axis=-1, keepdims=True)
    y = (xf - mu) * lax.rsqrt(var + EPS)
    return (y * g.astype(jnp.float32) + b.astype(jnp.float32)).astype(x.dtype)


def _causal_dwconv(x, w):
    K, C = w.shape
    return lax.conv_general_dilated(
        x, w[:, None, :].astype(x.dtype), window_strides=(1,), padding=[(K - 1, 0)],
        dimension_numbers=("NWC", "WIO", "NWC"), feature_group_count=C)


def _gla(q, k, v, g):
    B, S, H, dk = q.shape
    dv = v.shape[-1]
    n = S // GLA_CHUNK

    def to_chunks(t):
        return t.astype(jnp.float32).reshape(B, n, GLA_CHUNK, H, t.shape[-1]).transpose(1, 0, 3, 2, 4)

    qc = to_chunks(q * (dk ** -0.5))
    kc, vc, gc = to_chunks(k), to_chunks(v), to_chunks(g)
    causal = jnp.tril(jnp.ones((GLA_CHUNK, GLA_CHUNK), dtype=bool))

    def step(state, inp):
        qi, ki, vi, gi = inp
        b = jnp.cumsum(gi, axis=2)
        o_inter = jnp.einsum("bhik,bhkv->bhiv", qi * jnp.exp(b), state)
        diff = b[:, :, :, None, :] - b[:, :, None, :, :]
        decay = jnp.exp(jnp.where(causal[:, :, None], diff, -jnp.inf))
        scores = jnp.einsum("bhik,bhjk,bhijk->bhij", qi, ki, decay)
        o_intra = jnp.einsum("bhij,bhjv->bhiv", scores, vi)
        b_last = b[:, :, -1:, :]
        new_state = (jnp.exp(b_last[:, :, 0, :])[..., None] * state
                     + jnp.einsum("bhjk,bhjv->bhkv", ki * jnp.exp(b_last - b), vi))
        return new_state, o_inter + o_intra

    s0 = jnp.zeros((B, H, dk, dv), jnp.float32)
    _, o = lax.scan(step, s0, (qc, kc, vc, gc))
    return o.transpose(1, 0, 3, 2, 4).reshape(B, S, H, dv)


def _mixer(h, w_in, sc_w, cf_w, cf_b, cf_g, cf_beta, w_a2, b_a, gla_g, w_out):
    B, S, _ = h.shape
    p = h @ w_in
    split_idx = [int(i) for i in np.cumsum(IN_SIZES)[:-1]]
    sc_b, sc_c, sc_v, cf_a, cf_gate, q, k, v, a_lr, r = jnp.split(p, split_idx, axis=-1)

    y_sc = sc_b * _causal_dwconv(sc_c * sc_v, sc_w)

    u = cf_a * jax.nn.sigmoid(cf_gate)
    u = _causal_dwconv(u, cf_w) + cf_b
    y_cf = jax.nn.silu(_layernorm(u, cf_g, cf_beta))

    log_a = jax.nn.log_sigmoid((a_lr @ w_a2 + b_a).astype(jnp.float32)) / GLA_TAU
    o = _gla(q.reshape(B, S, GLA_H, GLA_HDK), k.reshape(B, S, GLA_H, GLA_HDK),
             v.reshape(B, S, GLA_H, GLA_HDV), log_a.reshape(B, S, GLA_H, GLA_HDK))
    o = _rmsnorm(o, gla_g).astype(h.dtype)
    y_gla = (o * jax.nn.silu(r).reshape(B, S, GLA_H, GLA_HDV)).reshape(B, S, GLA_DV)

    return jnp.concatenate([y_sc, y_cf, y_gla], axis=-1) @ w_out


def _swiglu(t, w_g, w_u, w_d):
    return (jax.nn.silu(t @ w_g) * (t @ w_u)) @ w_d


def _moe(h, w_router, w_g, w_u, w_d):
    B, S, D = h.shape
    t = h.reshape(-1, D)
    logits = (t @ w_router).astype(jnp.float32)
    top_v, top_i = lax.top_k(logits, TOP_K)
    top_w = jax.nn.softmax(top_v, axis=-1)
    gates = jnp.sum(jax.nn.one_hot(top_i, N_EXPERTS, dtype=jnp.float32) * top_w[..., None], axis=1)
    out = jnp.zeros(t.shape, jnp.float32)
    for e in range(N_EXPERTS):
        out = out + gates[:, e:e + 1] * _swiglu(t, w_g[e], w_u[e], w_d[e]).astype(jnp.float32)
    return out.astype(h.dtype).reshape(B, S, D)


def setup_inputs(seed: int = 0) -> dict:
    key = jax.random.key(seed)
    ks = jax.random.split(key, 24)
    nrm = lambda k, shape, scale: jax.random.normal(k, shape, jnp.float32) * scale
    gain = lambda k, shape: 1.0 + 0.01 * jax.random.normal(k, shape, jnp.float32)
    return {
        "x": jax.random.normal(ks[0], (BATCH, SEQ, D_MODEL), jnp.float32),
        "attn_norm_g": gain(ks[1], (DEPTH, D_MODEL)),
        "w_in": nrm(ks[2], (DEPTH, D_MODEL, D_IN), D_MODEL ** -0.5),
        "sc_conv_w": nrm(ks[3], (DEPTH, SC_K, SC_W), SC_K ** -0.5),
        "cf_conv_w": nrm(ks[4], (DEPTH, CF_K, CF_W), CF_K ** -0.5),
        "cf_conv_b": nrm(ks[5], (DEPTH, CF_W), 0.01),
        "cf_ln_g": gain(ks[6], (DEPTH, CF_W)),
        "cf_ln_b": nrm(ks[7], (DEPTH, CF_W), 0.01),
        "gla_w_a2": nrm(ks[8], (DEPTH, GLA_RANK, GLA_DK), GLA_RANK ** -0.5),
        "gla_b_a": nrm(ks[9], (DEPTH, GLA_DK), 0.01),
        "gla_norm_g": gain(ks[10], (DEPTH, GLA_H, GLA_HDV)),
        "w_out": nrm(ks[11], (DEPTH, D_MIX, D_MODEL), D_MIX ** -0.5),
        "ffn_norm_g": gain(ks[12], (DEPTH, D_MODEL)),
        "dense_w_gate": nrm(ks[13], (N_DENSE, D_MODEL, D_FF), D_MODEL ** -0.5),
        "dense_w_up": nrm(ks[14], (N_DENSE, D_MODEL, D_FF), D_MODEL ** -0.5),
        "dense_w_down": nrm(ks[15], (N_DENSE, D_FF, D_MODEL), D_FF ** -0.5),
        "moe_w_router": nrm(ks[16], (N_MOE, D_MODEL, N_EXPERTS), D_MODEL ** -0.5),
        "moe_w_gate": nrm(ks[17], (N_MOE, N_EXPERTS, D_MODEL, D_FF), D_MODEL ** -0.5),
        "moe_w_up": nrm(ks[18], (N_MOE, N_EXPERTS, D_MODEL, D_FF), D_MODEL ** -0.5),
        "moe_w_down": nrm(ks[19], (N_MOE, N_EXPERTS, D_FF, D_MODEL), D_FF ** -0.5),
        "final_norm_g": gain(ks[20], (D_MODEL,)),
    }


def reference(x, attn_norm_g, w_in, sc_conv_w, cf_conv_w, cf_conv_b, cf_ln_g, cf_ln_b,
              gla_w_a2, gla_b_a, gla_norm_g, w_out, ffn_norm_g,
              dense_w_gate, dense_w_up, dense_w_down,
              moe_w_router, moe_w_gate, moe_w_up, moe_w_down, final_norm_g):
    h = x
    for layer in range(DEPTH):
        a = _rmsnorm(h, attn_norm_g[layer])
        h = h + _mixer(a, w_in[layer], sc_conv_w[layer], cf_conv_w[layer], cf_conv_b[layer],
                       cf_ln_g[layer], cf_ln_b[layer], gla_w_a2[layer], gla_b_a[layer],
                       gla_norm_g[layer], w_out[layer])
        f = _rmsnorm(h, ffn_norm_g[layer])
        j = layer // 2
        if layer % 2 == 0:
            h = h + _swiglu(f, dense_w_gate[j], dense_w_up[j], dense_w_down[j])
        else:
            h = h + _moe(f, moe_w_router[j], moe_w_gate[j], moe_w_up[j], moe_w_down[j])
    return _rmsnorm(h, final_norm_g)
```

```python
import contextlib
import os
DBG_EX = os.environ.get('DBG_EX', '')
DBG_MB = int(os.environ.get('DBG_MB', '99'))
DBG_G = os.environ.get('DBG_G', '')
DBG_H = os.environ.get('DBG_H', '')
DBG_Y = os.environ.get('DBG_Y', '')
DBG_STOP = os.environ.get('DBG_STOP', 'final')
import numpy as np
import concourse.bass as bass
import concourse.mybir as mybir
from concourse.bass_utils import run_bass_kernel_spmd

F32 = mybir.dt.float32
BF16 = mybir.dt.bfloat16
AF = mybir.ActivationFunctionType
ALU = mybir.AluOpType
AX = mybir.AxisListType

NCORES = 8
D = 1024
T = 2048
NT = 16
TB = 256
NBLK = T // TB
HALO = 32
EXT = HALO + TB
D_IN = 2832
D_FF = 2816
NE = 8
EPS = 1e-6
O_SCB, O_SCC, O_SCV, O_CFA, O_CFG, O_Q, O_K, O_V, O_ALR, O_R = 0, 256, 512, 768, 1024, 1280, 1536, 1792, 2304, 2320
FF_GROUPS = [(0, 4), (4, 4), (8, 4), (12, 4), (16, 4), (20, 2)]
PV_SCW, PV_CFW, PV_CFB, PV_LNG, PV_LNB, PV_BA, PV_N = 0, 6, 68, 70, 72, 74, 76
C_ID, C_ONES, C_TRI, C_SM, C_N = 0, 128, 256, 320, 576


class DSem:
    def __init__(self, name):
        self.name = name
        self.count = 0
        self.handle = None


class Op:
    __slots__ = ("eng", "fn", "waits", "mark", "semval", "idx", "dma")

    def __init__(self, eng, fn, dma):
        self.eng, self.fn, self.dma = eng, fn, dma
        self.waits = []
        self.mark = False
        self.semval = None
        self.idx = 0


class Sched:
    ENGS = ["pe", "act", "dve", "pool", "sp"]

    def __init__(self):
        self.ops = {e: [] for e in self.ENGS}
        self.res = {}
        self.pending = {e: [] for e in self.ENGS}
        self.dsems = []

    def dsem(self, name):
        s = DSem(name)
        self.dsems.append(s)
        return s

    def add(self, eng, fn, r=(), w=(), dma=None, rg=None):
        op = Op(eng, fn, dma)
        op.idx = len(self.ops[eng])
        deps = []
        force = None
        if eng == "pe":
            prev = getattr(self, "_pe_rg", None)
            if rg is not None and prev is not None and (rg[0] + rg[1] <= prev[0] or prev[0] + prev[1] <= rg[0]):
                force = self.ops["pe"][-1]
            self._pe_rg = rg
        for x in r:
            st = self.res.get(x)
            if st is not None and st[0] is not None:
                deps.append(st[0])
        for x in w:
            st = self.res.get(x)
            if st is not None:
                if st[0] is not None:
                    deps.append(st[0])
                deps.extend(st[1])
        deps.extend(self.pending[eng])
        self.pending[eng] = []
        if force is not None:
            force.mark = True
            op.waits.append(("op", force))
        seen = set()
        for tok in deps:
            if id(tok) in seen:
                continue
            seen.add(id(tok))
            if tok[0] == "op":
                p = tok[1]
                if p.eng == eng:
                    if eng == "pe":
                        continue
                    if dma is None and (op.idx - p.idx) > 2:
                        continue
                p.mark = True
            op.waits.append(tok)
        if dma is not None:
            dma.count += 1
            tok = ("dma", dma, dma.count * 16)
        else:
            tok = ("op", op)
        for x in r:
            st = self.res.setdefault(x, [None, []])
            st[1].append(tok)
        for x in w:
            self.res[x] = [tok, []]
        self.ops[eng].append(op)
        return op

    def barrier(self):
        toks = []
        for e in self.ENGS:
            if self.ops[e]:
                last = None
                for o in reversed(self.ops[e]):
                    if o.dma is None and o.fn is not None:
                        last = o
                        break
                if last is not None:
                    last.mark = True
                    toks.append(("op", last))
        for s in self.dsems:
            if s.count:
                toks.append(("dma", s, s.count * 16))
        for e in self.ENGS:
            self.pending[e] = [t for t in toks if not (t[0] == "op" and t[1].eng == e and e == "pe")]
        self.res = {}

    def emit(self, nc, stack):
        EPOCH = 30000
        engsems = {}
        for e in self.ENGS:
            n = 0
            for o in self.ops[e]:
                if o.mark:
                    n += 1
                    o.semval = n
            nep = max(1, (n + EPOCH - 1) // EPOCH)
            engsems[e] = [stack.enter_context(nc.semaphore(f"s_{e}{i}")) for i in range(nep)]
        for s in self.dsems:
            if s.count:
                s.handle = stack.enter_context(nc.semaphore("d_" + s.name))

        def semof(e, val):
            i = (val - 1) // EPOCH
            return engsems[e][i], val - i * EPOCH

        block = stack.enter_context(nc.Block())

        def run(ename):
            def body(eng):
                waited = {}
                for o in self.ops[ename]:
                    for tok in o.waits:
                        if tok[0] == "op":
                            sem, val = semof(tok[1].eng, tok[1].semval)
                        else:
                            sem, val = tok[1].handle, tok[2]
                        key = id(sem)
                        if waited.get(key, 0) >= val:
                            continue
                        waited[key] = val
                        eng.wait_ge(sem, val)
                    if o.fn is None:
                        continue
                    ins = o.fn(eng)
                    if o.dma is not None:
                        ins.then_inc(o.dma.handle, 16)
                    elif o.mark:
                        sem, _ = semof(ename, o.semval)
                        ins.then_inc(sem, 1)
            return body

        block.tensor(run("pe"))
        block.scalar(run("act"))
        block.vector(run("dve"))
        block.gpsimd(run("pool"))
        block.sync(run("sp"))


STAGES = ['load', 'mixA', 'ffnA', 'preA', 'mix0', 'ffn0', 'mix1', 'ffn1', 'final']


def build_program(stop='final'):
    nc = bass.Bass("TRN2", target_bir_lowering=False)
    S = Sched()
    SI = STAGES.index(stop)
    need_moe = SI >= STAGES.index('ffn1')

    def din(name, shape):
        return nc.dram_tensor(name, list(shape), F32, kind="ExternalInput").ap()

    x_d = din("x", [T, D])
    xp_d = din("x_prev", [T, D])
    sel_d = din("sel", [128, NCORES])
    consts_d = din("consts", [128, C_N])
    pvec_d = din("pvec", [2, 128, PV_N])
    bvec_d = din("bvec", [5, 128, D])
    gng_d = din("gng", [2, 128, 512])
    wa2_d = din("w_a2", [2, 16, 256])
    win_d = din("w_in", [2, D, D_IN])
    wout_d = din("w_out", [2, D, D])
    dwg_d = din("dense_w_gate", [1, D, D_FF])
    dwu_d = din("dense_w_up", [1, D, D_FF])
    dwd_d = din("dense_w_down", [1, D_FF, D])
    wr_d = din("moe_w_router", [1, D, NE])
    if need_moe:
        mwg_d = din("moe_w_gate", [1, NE, D, D_FF])
        mwu_d = din("moe_w_up", [1, NE, D, D_FF])
        mwd_d = din("moe_w_down", [1, NE, D_FF, D])
    y_d = nc.dram_tensor("y", [T, D], F32, kind="ExternalOutput").ap()

    stack = contextlib.ExitStack()
    with stack:
        def sb(name, shape, dt=F32):
            return stack.enter_context(nc.sbuf_tensor("sb_" + name, list(shape), dt))

        h = sb("h", [128, NT, D])
        wbuf = sb("wbuf", [128, 8 * D_IN + 8 * D], BF16)
        W_in = wbuf[:, 0:8 * D_IN].rearrange("p (k c) -> p k c", k=8)
        W_out = wbuf[:, 8 * D_IN:8 * D_IN + 8 * D].rearrange("p (k c) -> p k c", k=8)
        FW = []
        for s_ in range(2):
            base = s_ * 12288
            wg = wbuf[:, base:base + 4096].rearrange("p (k c) -> p k c", k=8)
            wu = wbuf[:, base + 4096:base + 8192].rearrange("p (k c) -> p k c", k=8)
            wd = wbuf[:, base + 8192:base + 12288].rearrange("p (g c) -> p g c", g=4)
            FW.append((wg, wu, wd))
        RR_N = 10240
        RR = sb("RR", [128, RR_N], F32)
        fT = RR[:, 0:8192].bitcast(BF16).rearrange("p (k t) -> p k t", k=8)
        _off = [0]

        def carve(n_f32, dt=F32, shape=None):
            a = RR[:, _off[0]:_off[0] + n_f32]
            _off[0] += n_f32
            if dt == BF16:
                a = a.bitcast(BF16)
            return a

        S_A = carve(256).rearrange("p (a t) -> p a t", a=2)
        S_T = carve(256).rearrange("p (a t) -> p a t", a=2)
        S_bf = carve(128, BF16).rearrange("p (a t) -> p a t", a=2)
        _x0 = _off[0]
        csp = carve(512).rearrange("p (a t) -> p a t", a=2)
        E_ = carve(512).rearrange("p (a t) -> p a t", a=2)
        Ei = carve(512).rearrange("p (a t) -> p a t", a=2)
        qtT = carve(256, BF16).rearrange("p (a t) -> p a t", a=2)
        ktT = carve(256, BF16).rearrange("p (a t) -> p a t", a=2)
        kt = carve(256, BF16).rearrange("p (a t) -> p a t", a=2)
        vv = carve(512, BF16).rearrange("p (a t) -> p a t", a=2)
        sr = carve(512, BF16).rearrange("p (a t) -> p a t", a=2)
        m_ = carve(2 * EXT).rearrange("p (a t) -> p a t", a=2)
        u_ = carve(EXT, BF16).rearrange("p (a t) -> p a t", a=2)
        tmpx = carve(EXT)
        acc_sc = carve(256)
        cfc = carve(512).rearrange("p (a t) -> p a t", a=2)
        cfsq = carve(512).rearrange("p (a t) -> p a t", a=2)
        mean_sb = carve(256)
        var_sb = carve(256)
        rstd_sb = carve(256)
        yT = carve(1024, BF16).rearrange("p (a t) -> p a t", a=8)
        o_sb = carve(512)
        og = carve(512)
        ogb = carve(256, BF16)
        AT = carve(128, BF16).rearrange("p (a t) -> p a t", a=4)
        alrT = carve(256)
        assert _off[0] <= RR_N, _off[0]
        _off[0] = _x0
        stS = carve(256)
        stH = carve(1024)
        haloh = carve(1024)
        outst = RR[:, 0:2048].rearrange("p (a t) -> p a t", a=2)

        aTx = sb("aTx", [128, 8, EXT], BF16)
        FR = sb("FR", [128, 4608], F32)
        actT = FR[:, 0:2048].bitcast(BF16).rearrange("p (a j t) -> p a j t", a=2, j=4)
        f32t = FR[:, 2048:3072]
        f32T = FR[:, 3072:4096].rearrange("p (k t) -> p k t", k=8)
        sg = FR[:, 4096:4608].bitcast(BF16).rearrange("p (a t) -> p a t", a=2)
        D_cf = FR[:, 0:3968].bitcast(BF16).rearrange("p (j c) -> p j c", j=62)
        aTx1 = FR[:, 0:1152].bitcast(BF16).rearrange("p (k t) -> p k t", k=8)
        csp1 = FR[:, 1152:1664].rearrange("p (a t) -> p a t", a=2)
        E_1 = FR[:, 1664:2176].rearrange("p (a t) -> p a t", a=2)
        Ei1 = FR[:, 2176:2688].rearrange("p (a t) -> p a t", a=2)
        a_bf = sb("a_bf", [128, D], BF16)
        gvec = sb("gvec", [128, D])
        consts = sb("consts", [128, C_N])
        ident_bf = sb("ident_bf", [128, 128], BF16)
        pvec = sb("pvec", [128, 2, PV_N])
        nba = sb("nba", [128, 2, 2])
        gng = sb("gng", [128, 2, 512])
        wa2 = sb("wa2", [16, 2, 256])
        wr = sb("wr", [128, 8, NE])
        sel = sb("sel", [128, NCORES])
        small = sb("small", [128, 64])
        Ssave = sb("Ssave", [128, 2, 256])
        Hsave = sb("Hsave", [128, 2, 8 * HALO], BF16)
        gates = sb("gates", [128, NT, NE])

        psF = [stack.enter_context(nc.psum_tensor(f"psF{i}", [128, 512], F32)) for i in range(6)]
        psB = [stack.enter_context(nc.psum_tensor(f"psB{i}", [128, 1024], BF16)) for i in range(2)]
        _rr = [0, 0]

        def nextF():
            i = _rr[0] % 5
            _rr[0] += 1
            return psF[i], f"F{i}"

        def nextB():
            i = _rr[1] % 2
            _rr[1] += 1
            return psB[i], f"B{i}"

        ident = consts[:, C_ID:C_ID + 128]
        ones256 = consts[:, C_ONES:C_ONES + 128]
        tri = consts[:, C_TRI:C_TRI + 64]
        smask = consts[:, C_SM:C_SM + 256]

        def mm(out, lhsT, rhs, start, stop, r, w):
            rg = (lhsT.base_partition(), lhsT.partition_size())
            S.add("pe", lambda e: e.matmul(out, lhsT=lhsT, rhs=rhs, start=start, stop=stop), r=r, w=w, rg=rg)

        def tr(out, in_, idn, r, w):
            rg = (in_.base_partition(), in_.partition_size())
            S.add("pe", lambda e: e.transpose(out=out, in_=in_, identity=idn), r=r, w=w, rg=rg)

        def act(out, in_, func, r, w, bias=None, scale=None, accum=None):
            kw = {}
            if bias is not None:
                kw["bias"] = bias
            if scale is not None:
                kw["scale"] = scale
            if accum is not None:
                kw["accum_out"] = accum
            S.add("act", lambda e: e.activation(out=out, in_=in_, func=func, **kw), r=r, w=w)

        def tt(out, in0, in1, op, r, w, eng="dve"):
            S.add(eng, lambda e: e.tensor_tensor(out=out, in0=in0, in1=in1, op=op), r=r, w=w)

        def ts(out, in0, s1, op0, r, w, s2=None, op1=None, eng="dve"):
            if op1 is None:
                S.add(eng, lambda e: e.tensor_scalar(out=out, in0=in0, scalar1=s1, scalar2=None, op0=op0), r=r, w=w)
            else:
                S.add(eng, lambda e: e.tensor_scalar(out=out, in0=in0, scalar1=s1, scalar2=s2, op0=op0, op1=op1), r=r, w=w)

        def stt(out, in0, scalar, in1, op0, op1, r, w):
            S.add("dve", lambda e: e.scalar_tensor_tensor(out=out, in0=in0, scalar=scalar, in1=in1, op0=op0, op1=op1), r=r, w=w)

        def cp(out, in_, r, w, eng="dve"):
            S.add(eng, lambda e: e.tensor_copy(out=out, in_=in_), r=r, w=w)

        def dma(q, out, in_, r, w, sem):
            S.add(q, lambda e: e.dma_start(out=out, in_=in_), r=r, w=w, dma=sem)

        s_misc = [S.dsem(f"misc{i}") for i in range(10)]
        dma("sp", consts[:], consts_d, [], ["consts"], s_misc[0])
        dma("pool", ident_bf[:], consts_d[:, C_ID:C_ID + 128], [], ["ident_bf"], s_misc[1])
        dma("sp", pvec[:], pvec_d.rearrange("l p n -> p l n"), [], ["pvec"], s_misc[2])
        dma("sp", gng[:], gng_d.rearrange("l p n -> p l n"), [], ["gng"], s_misc[3])
        dma("sp", wa2[:], wa2_d.rearrange("l r n -> r l n"), [], ["wa2"], s_misc[4])
        dma("sp", wr[:], wr_d[0].rearrange("(k p) e -> p k e", p=128), [], ["wr"], s_misc[5])
        dma("sp", sel[:], sel_d, [], ["sel"], s_misc[6])
        s_x = [S.dsem(f"x{t}") for t in range(NT)]

        def load_x(src):
            for t in range(NT):
                dma("sp", h[:, t, :], src[t * 128:(t + 1) * 128, :], [], [f"h{t}"], s_x[t])
        ts(nba[:], pvec[:, :, PV_BA:PV_BA + 2], -1.0, ALU.mult, ["pvec"], ["nba"])

        s_win = [S.dsem(f"win{k}") for k in range(8)]
        s_wout = S.dsem("wout")
        s_gv = S.dsem("gv")
        s_fw = [[S.dsem(f"fw{s_}{j}") for j in range(3)] for s_ in range(2)]

        def load_mixer_weights(l):
            for k in range(8):
                dma("pool", W_in[:, k, :], win_d[l, k * 128:(k + 1) * 128, :], [], [f"Win{k}"] + [f"fw{s_}{j}" for s_ in range(2) for j in range(3)], s_win[k])
            dma("pool", W_out[:], wout_d[l].rearrange("(k p) c -> p k c", p=128), [], ["Wout"], s_wout)

        def norm_tile(src, src_res, nrows, gidx_res, dstT, dst_res, want_f32=False):
            ss = small[0:nrows, 0:1]
            lnv = small[0:nrows, 1:2]
            rstd = small[0:nrows, 2:3]
            act(a_bf[0:nrows, :], src, AF.Square, [src_res], ["a_bf", "ss"], accum=ss)
            act(lnv, ss, AF.Ln, ["ss"], ["lnv"], bias=EPS, scale=1.0 / D)
            act(rstd, lnv, AF.Exp, ["lnv"], ["rstd"], scale=-0.5)
            if want_f32:
                stt(f32t[0:nrows, :], src, rstd, gvec[0:nrows, :], ALU.mult, ALU.mult, [src_res, "rstd", gidx_res], ["f32t"])
                act(a_bf[0:nrows, :], f32t[0:nrows, :], AF.Copy, ["f32t"], ["a_bf"])
            else:
                stt(a_bf[0:nrows, :], src, rstd, gvec[0:nrows, :], ALU.mult, ALU.mult, [src_res, "rstd", gidx_res], ["a_bf"])
            pb, pbn = nextB()
            for k in range(8):
                tr(pb[:, k * nrows:(k + 1) * nrows], a_bf[0:nrows, k * 128:(k + 1) * 128], ident_bf[0:nrows, 0:nrows],
                   ["a_bf", "ident_bf"], [pbn])
            cp(dstT, pb[:, 0:8 * nrows].rearrange("p (k t) -> p k t", k=8), [pbn], [dst_res], eng="act" if False else "dve")

        def load_gvec(i):
            dma("sp", gvec[:], bvec_d[i], [], ["gvec"], s_gv)

        class BS:
            pass

        PB = []
        for i_, (a_, c_, e_, ei_) in enumerate([(aTx, csp, E_, Ei), (aTx, csp, E_, Ei)]):
            o_ = BS()
            o_.aTx, o_.csp, o_.E, o_.Ei = a_, c_, e_, ei_
            o_.n_aTx, o_.n_csp, o_.n_E, o_.n_Ei = "aTx0", "csp0", "E0", "Ei0"
            PB.append(o_)

        def proj_fm(col0, ncols, rhs, rhs_res, N):
            p, pn = nextF()
            for k in range(8):
                mm(p[0:ncols, 0:N], W_in[:, k, col0:col0 + ncols], rhs[:, k, :], k == 0, k == 7, [f"Win{k}", rhs_res], [pn])
            return p, pn

        def prep_stream(l, b, P, Pprev):
            if Pprev is not None:
                cp(P.aTx[:, :, 0:HALO], Pprev.aTx[:, :, EXT - HALO:EXT], [Pprev.n_aTx], [P.n_aTx])
            for ti in range(2):
                t = b * 2 + ti
                norm_tile(h[:, t, :], f"h{t}", 128, "gvec", P.aTx[:, :, HALO + ti * 128:HALO + (ti + 1) * 128], P.n_aTx)
                yield
            rhs = P.aTx[:, :, HALO:EXT]
            p, pn = proj_fm(O_ALR, 16, rhs, P.n_aTx, TB)
            cp(alrT[0:16, :], p[0:16, 0:TB], [pn], ["alrT"])
            yield
            pz, pzn = nextF()
            for hp in range(2):
                mm(pz[:, hp * 256:(hp + 1) * 256], wa2[0:16, l, hp * 128:(hp + 1) * 128], alrT[0:16, :], True, True, ["wa2", "alrT"], [pzn])
            for hp in range(2):
                act(P.csp[:, hp, :], pz[:, hp * 256:(hp + 1) * 256], AF.Exp, [pzn, "nba"], [P.n_csp], bias=nba[:, l, hp:hp + 1], scale=-1.0)
            yield
            act(P.csp[:], P.csp[:], AF.Ln, [P.n_csp], [P.n_csp], bias=1.0)
            for hp in range(2):
                S.add("dve", (lambda hp_: (lambda e: e.tensor_tensor_scan(out=P.csp[:, hp_, :], data0=smask, data1=P.csp[:, hp_, :], initial=0.0, op0=ALU.mult, op1=ALU.add)))(hp),
                      r=[P.n_csp, "consts"], w=[P.n_csp])
            yield
            act(P.Ei[:], P.csp[:], AF.Exp, [P.n_csp], [P.n_Ei], scale=1.0 / 16.0)
            act(P.E[:], P.csp[:], AF.Exp, [P.n_csp], [P.n_E], scale=-1.0 / 16.0)
            yield

        def kv_block(P):
            rhs = P.aTx[:, :, HALO:EXT]
            pk, pkn = nextF()
            for hp in range(2):
                for k in range(8):
                    mm(pk[:, hp * 256:(hp + 1) * 256], W_in[:, k, O_K + hp * 128:O_K + (hp + 1) * 128], rhs[:, k, :], k == 0, k == 7, [f"Win{k}", P.n_aTx], [pkn])
            tt(ktT[:], pk[:].rearrange("p (a t) -> p a t", a=2), P.Ei[:], ALU.mult, [pkn, P.n_Ei], ["ktT"])
            pb, pbn = nextB()
            for ti in range(2):
                for hp in range(2):
                    tr(pb[:, ti * 256 + hp * 128: ti * 256 + (hp + 1) * 128], ktT[:, hp, ti * 128:(ti + 1) * 128], ident_bf[:], ["ktT", "ident_bf"], [pbn])
            cp(kt[:], pb[:, 0:512].rearrange("p (a t) -> p a t", a=2), [pbn], ["kt"])
            for ti in range(2):
                pv, pvn = nextF()
                for k in range(8):
                    mm(pv[:], P.aTx[:, k, HALO + ti * 128:HALO + (ti + 1) * 128], W_in[:, k, O_V:O_V + 512], k == 0, k == 7, [f"Win{k}", P.n_aTx], [pvn])
                act(vv[:, ti, :], pv[:], AF.Copy, [pvn], ["vv"])

        def state_update(c, P):
            ti, par = c // 2, c % 2
            rows = slice(par * 64, par * 64 + 64)
            pS, pSn = nextF()
            for hh in range(4):
                hp, hl = hh // 2, hh % 2
                mm(pS[hl * 64:(hl + 1) * 64, hp * 128:(hp + 1) * 128], kt[rows, ti, hh * 64:(hh + 1) * 64], vv[rows, ti, hh * 128:(hh + 1) * 128],
                   True, True, ["kt", "vv"], [pSn])
            tt(S_T[:], pS[:, 0:256].rearrange("p (a t) -> p a t", a=2), S_A[:], ALU.add, [pSn, "S_A"], ["S_T"])
            for hp in range(2):
                ts(S_A[:, hp, :], S_T[:, hp, :], P.E[:, hp, c * 64 + 63:c * 64 + 64], ALU.mult, ["S_T", P.n_E], ["S_A"])
            cp(S_bf[:], S_A[:], ["S_A"], ["S_bf"])

        def prepass_stream(P):
            kv_block(P)
            yield
            for c in range(4):
                state_update(c, P)
                yield

        def interleave(*gens):
            gens = [g_ for g_ in gens if g_ is not None]
            while gens:
                for g_ in list(gens):
                    try:
                        next(g_)
                    except StopIteration:
                        gens.remove(g_)

        def conv_stream(l, b, P):
            aX, nX = P.aTx, P.n_aTx
            rhs = aX[:, :, HALO:EXT]
            for cc in range(2):
                pc, pcn = nextF()
                for k in range(8):
                    mm(pc[:, 0:EXT], W_in[:, k, O_SCC + cc * 128:O_SCC + (cc + 1) * 128], aX[:, k, :], k == 0, k == 7, [f"Win{k}", nX], [pcn])
                pv, pvn = nextF()
                for k in range(8):
                    mm(pv[:, 0:EXT], W_in[:, k, O_SCV + cc * 128:O_SCV + (cc + 1) * 128], aX[:, k, :], k == 0, k == 7, [f"Win{k}", nX], [pvn])
                act(tmpx[:], pc[:, 0:EXT], AF.Copy, [pcn], ["tmpx"])
                tt(m_[:, cc, :], tmpx[:], pv[:, 0:EXT], ALU.mult, ["tmpx", pvn], ["m"])
                yield
                w0 = pvec[:, l, PV_SCW + cc * 3 + 0:PV_SCW + cc * 3 + 1]
                w1 = pvec[:, l, PV_SCW + cc * 3 + 1:PV_SCW + cc * 3 + 2]
                w2 = pvec[:, l, PV_SCW + cc * 3 + 2:PV_SCW + cc * 3 + 3]
                ts(acc_sc[:], m_[:, cc, HALO - 2:HALO - 2 + TB], w0, ALU.mult, ["m", "pvec"], ["acc_sc"])
                stt(acc_sc[:], m_[:, cc, HALO - 1:HALO - 1 + TB], w1, acc_sc[:], ALU.mult, ALU.add, ["m", "pvec", "acc_sc"], ["acc_sc"])
                stt(acc_sc[:], m_[:, cc, HALO:HALO + TB], w2, acc_sc[:], ALU.mult, ALU.add, ["m", "pvec", "acc_sc"], ["acc_sc"])
                pb_, pbn_ = proj_fm(O_SCB + cc * 128, 128, rhs, nX, TB)
                tt(yT[:, cc, :], acc_sc[:], pb_[:, 0:TB], ALU.mult, ["acc_sc", pbn_], ["yT_sc"])
                yield
            for cc in range(2):
                pa, pan = nextF()
                for k in range(8):
                    mm(pa[:, 0:EXT], W_in[:, k, O_CFA + cc * 128:O_CFA + (cc + 1) * 128], aX[:, k, :], k == 0, k == 7, [f"Win{k}", nX], [pan])
                pg, pgn = nextF()
                for k in range(8):
                    mm(pg[:, 0:EXT], W_in[:, k, O_CFG + cc * 128:O_CFG + (cc + 1) * 128], aX[:, k, :], k == 0, k == 7, [f"Win{k}", nX], [pgn])
                act(tmpx[:], pg[:, 0:EXT], AF.Tanh, [pgn], ["tmpx"], scale=0.5)
                stt(u_[:, cc, :], tmpx[:], 1.0, pa[:, 0:EXT], ALU.add, ALU.mult, ["tmpx", pan], ["u"])
                yield
                pcv, pcvn = nextF()
                for kk in range(31):
                    mm(pcv[:, 0:TB], D_cf[:, cc * 31 + kk, :], u_[:, cc, HALO - 30 + kk:HALO - 30 + kk + TB], kk == 0, kk == 30, ["Dcf", "u"], [pcvn])
                act(cfc[:, cc, :], pcv[:, 0:TB], AF.Identity, [pcvn, "pvec"], ["cfc"], bias=pvec[:, l, PV_CFB + cc:PV_CFB + cc + 1], scale=0.5)
                act(cfsq[:, cc, :], cfc[:, cc, :], AF.Square, ["cfc"], ["cfsq"])
                yield
            pm, pmn = nextF()
            for cc in range(2):
                mm(pm[:, 0:TB], ones256, cfc[:, cc, :], cc == 0, cc == 1, ["consts", "cfc"], [pmn])
            for cc in range(2):
                mm(pm[:, TB:2 * TB], ones256, cfsq[:, cc, :], cc == 0, cc == 1, ["consts", "cfsq"], [pmn])
            cp(mean_sb[:], pm[:, 0:TB], [pmn], ["mean"])
            tt(var_sb[:], mean_sb[:], mean_sb[:], ALU.mult, ["mean"], ["var"])
            tt(var_sb[:], pm[:, TB:2 * TB], var_sb[:], ALU.subtract, [pmn, "var"], ["var"])
            yield
            act(rstd_sb[:], var_sb[:], AF.Ln, ["var"], ["rstdc"], bias=EPS)
            act(rstd_sb[:], rstd_sb[:], AF.Exp, ["rstdc"], ["rstdc"], scale=-0.5)
            for cc in range(2):
                tt(cfc[:, cc, :], cfc[:, cc, :], mean_sb[:], ALU.subtract, ["cfc", "mean"], ["cfc"])
                tt(cfc[:, cc, :], cfc[:, cc, :], rstd_sb[:], ALU.mult, ["cfc", "rstdc"], ["cfc"])
                act(yT[:, 2 + cc, :], cfc[:, cc, :], AF.Silu, ["cfc", "pvec"], ["yT_cf"],
                    bias=pvec[:, l, PV_LNB + cc:PV_LNB + cc + 1], scale=pvec[:, l, PV_LNG + cc:PV_LNG + cc + 1])
                yield

        def gla_stream(l, b, P):
            aX, nX = P.aTx, P.n_aTx
            rhs = aX[:, :, HALO:EXT]
            pq, pqn = nextF()
            for hp in range(2):
                for k in range(8):
                    mm(pq[:, hp * 256:(hp + 1) * 256], W_in[:, k, O_Q + hp * 128:O_Q + (hp + 1) * 128], rhs[:, k, :], k == 0, k == 7, [f"Win{k}", nX], [pqn])
            stt(qtT[:], pq[:].rearrange("p (a t) -> p a t", a=2), 0.125, P.E[:], ALU.mult, ALU.mult, [pqn, P.n_E], ["qtT"])
            yield
            kv_block(P)
            yield
            for ti in range(2):
                pr, prn = nextF()
                for k in range(8):
                    mm(pr[:], aX[:, k, HALO + ti * 128:HALO + (ti + 1) * 128], W_in[:, k, O_R:O_R + 512], k == 0, k == 7, [f"Win{k}", nX], [prn])
                act(sr[:, ti, :], pr[:], AF.Silu, [prn], ["sr"])
                yield
            po, pon = psF[5], "F5"
            for ti in range(2):
                for par in range(2):
                    c = ti * 2 + par
                    rows = slice(par * 64, par * 64 + 64)
                    cols = slice(c * 64, c * 64 + 64)
                    pA, pAn = nextF()
                    for hh in (0, 2, 1, 3):
                        hp, hl = hh // 2, hh % 2
                        fr = slice(hl * 64, hl * 64 + 64)
                        mm(pA[rows, hh * 64:(hh + 1) * 64], ktT[fr, hp, cols], qtT[fr, hp, cols], True, True, ["ktT", "qtT"], [pAn])
                    tt(AT[rows, :, :], pA[rows, 0:256].rearrange("p (a t) -> p a t", a=4), tri[rows, :].unsqueeze(1).to_broadcast([64, 4, 64]),
                       ALU.mult, [pAn, "consts"], ["AT"])
                    yield
                    for hh in ((0, 2, 1, 3) if par == 0 else (1, 3, 0, 2)):
                        hp, hl = hh // 2, hh % 2
                        fr = slice(hl * 64, hl * 64 + 64)
                        mm(po[rows, hh * 128:(hh + 1) * 128], qtT[fr, hp, cols], S_bf[fr, hp, :], True, False, ["qtT", "S_bf"], [pon])
                        mm(po[rows, hh * 128:(hh + 1) * 128], AT[rows, hh, :], vv[rows, ti, hh * 128:(hh + 1) * 128], False, True, ["AT", "vv"], [pon])
                    yield
                    state_update(c, P)
                    yield
                act(o_sb[:], po[:], AF.Copy, [pon], ["o_sb"])
                for hh in range(4):
                    act(og[:, hh * 128:(hh + 1) * 128], o_sb[:, hh * 128:(hh + 1) * 128], AF.Square, ["o_sb"], ["og", "ss4"], accum=small[:, 8 + hh:9 + hh])
                act(small[:, 12:16], small[:, 8:12], AF.Ln, ["ss4"], ["ln4"], bias=EPS, scale=1.0 / 128.0)
                act(small[:, 16:20], small[:, 12:16], AF.Exp, ["ln4"], ["rstd4"], scale=-0.5)
                yield
                for hh in range(4):
                    stt(og[:, hh * 128:(hh + 1) * 128], o_sb[:, hh * 128:(hh + 1) * 128], small[:, 16 + hh:17 + hh], gng[:, l, hh * 128:(hh + 1) * 128],
                        ALU.mult, ALU.mult, ["o_sb", "rstd4", "gng"], ["og"])
                tt(ogb[:], og[:], sr[:, ti, :], ALU.mult, ["og", "sr"], ["ogb"])
                pb, pbn = nextB()
                for j in range(4):
                    tr(pb[:, j * 128:(j + 1) * 128], ogb[:, j * 128:(j + 1) * 128], ident_bf[:], ["ogb", "ident_bf"], [pbn])
                cp(yT[:, 4:8, ti * 128:(ti + 1) * 128], pb[:, 0:512].rearrange("p (a t) -> p a t", a=4), [pbn], ["yT_gla"])
                yield

        def wout_block(b):
            for ti in range(2):
                t = b * 2 + ti
                for half in range(2):
                    pw, pwn = nextF()
                    for k in range(8):
                        mm(pw[:], yT[:, k, ti * 128:(ti + 1) * 128], W_out[:, k, half * 512:(half + 1) * 512], k == 0, k == 7,
                           ["yT_sc", "yT_cf", "yT_gla", "Wout"], [pwn])
                    tt(h[:, t, half * 512:(half + 1) * 512], pw[:], h[:, t, half * 512:(half + 1) * 512], ALU.add, [pwn, f"h{t}"], [f"h{t}"])
                    yield

        def save_state(i):
            P = PB[(NBLK - 1) % 2]
            ts(Ssave[:, i, :], S_A[:].rearrange("p a t -> p (a t)"), sel[:, 0:1], ALU.mult, ["S_A", "sel"], [f"Ssave{i}"])
            ts(Hsave[:, i, :].rearrange("p (k t) -> p k t", k=8), P.aTx[:, :, EXT - HALO:EXT], sel[:, 0:1], ALU.mult, [P.n_aTx, "sel"], [f"Hsave{i}"])

        def mixer_main(l, src):
            load_gvec(l)
            load_mixer_weights(l)
            if src is None:
                S.add("dve", lambda e: e.memset(S_A[:], 0.0), r=[], w=["S_A"])
                S.add("dve", lambda e: e.memset(PB[0].aTx[:, :, 0:HALO], 0.0), r=[], w=[PB[0].n_aTx])
            else:
                cp(S_A[:].rearrange("p a t -> p (a t)"), Ssave[:, src, :], [f"Ssave{src}"], ["S_A"])
                cp(PB[0].aTx[:, :, 0:HALO], Hsave[:, src, :].rearrange("p (k t) -> p k t", k=8), [f"Hsave{src}"], [PB[0].n_aTx])
            cp(S_bf[:], S_A[:], ["S_A"], ["S_bf"])
            for j in range(62):
                ts(D_cf[:, j, :], ident_bf[:], pvec[:, l, PV_CFW + j:PV_CFW + j + 1], ALU.mult, ["ident_bf", "pvec"], ["Dcf"])
            P = PB[0]
            interleave(prep_stream(l, 0, P, None))
            for b in range(NBLK):
                interleave(gla_stream(l, b, P), conv_stream(l, b, P))
                interleave(wout_block(b), prep_stream(l, b + 1, P, P) if b + 1 < NBLK else None)

        def mixer_prepass(l):
            load_gvec(l)
            load_mixer_weights(l)
            S.add("dve", lambda e: e.memset(S_A[:], 0.0), r=[], w=["S_A"])
            P = PB[0]
            for b in range(NBLK):
                interleave(prep_stream(l, b, P, P if b > 0 else None))
                interleave(prepass_stream(P))

        def ffn_norm(l, moe):
            load_gvec(2 + l)
            for t in range(NT):
                norm_tile(h[:, t, :], f"h{t}", 128, "gvec", fT[:, :, t * 128:(t + 1) * 128], "fT", want_f32=moe)
                if moe:
                    router_tile(t)

        def router_tile(t):
            pl, pln = nextF()
            for half in range(2):
                pt, ptn = nextF()
                for j in range(4):
                    k = half * 4 + j
                    tr(pt[:, j * 128:(j + 1) * 128], f32t[:, k * 128:(k + 1) * 128], ident, ["f32t", "consts"], [ptn])
                cp(f32T[:, half * 4:(half + 1) * 4, :], pt[:].rearrange("p (a t) -> p a t", a=4), [ptn], ["f32T"])
            for k in range(8):
                mm(pl[:, 0:NE], f32T[:, k, :], wr[:, k, :], k == 0, k == 7, ["f32T", "wr"], [pln])
            lg = small[:, 24:32]
            m1 = small[:, 32:33]
            m2 = small[:, 33:34]
            eq = small[:, 34:42]
            l2 = small[:, 42:50]
            ex = small[:, 50:58]
            den = small[:, 58:59]
            nm1 = small[:, 59:60]
            cp(lg, pl[:, 0:NE], [pln], ["lg"])
            S.add("dve", lambda e: e.reduce_max(out=m1, in_=lg, axis=AX.X), r=["lg"], w=["m1"])
            ts(eq, lg, m1, ALU.is_equal, ["lg", "m1"], ["eq"])
            stt(l2, eq, -1e30, lg, ALU.mult, ALU.add, ["eq", "lg"], ["l2"])
            S.add("dve", lambda e: e.reduce_max(out=m2, in_=l2, axis=AX.X), r=["l2"], w=["m2"])
            ts(eq, lg, m2, ALU.is_ge, ["lg", "m2"], ["eq"])
            ts(nm1, m1, -1.0, ALU.mult, ["m1"], ["nm1"])
            act(ex, lg, AF.Exp, ["lg", "nm1"], ["ex"], bias=nm1, scale=1.0)
            tt(ex, ex, eq, ALU.mult, ["ex", "eq"], ["ex"])
            S.add("dve", lambda e: e.reduce_sum(out=den, in_=ex, axis=AX.X), r=["ex"], w=["den"])
            S.add("dve", lambda e: e.reciprocal(out=den, in_=den), r=["den"], w=["den"])
            ts(gates[:, t, :], ex, den, ALU.mult, ["ex", "den"], ["gates"])

        _fwi = [0]

        def ffn_group(wg_src, wu_src, wd_src, c0, G, gate_col):
            s_ = _fwi[0] % 2
            _fwi[0] += 1
            wg, wu, wd = FW[s_]
            dma("pool", wg[:, :, 0:G * 128], wg_src.rearrange("(k p) c -> p k c", p=128)[:, :, c0 * 128:(c0 + G) * 128], [], [f"fw{s_}0"], s_fw[s_][0])
            dma("pool", wu[:, :, 0:G * 128], wu_src.rearrange("(k p) c -> p k c", p=128)[:, :, c0 * 128:(c0 + G) * 128], [], [f"fw{s_}1"], s_fw[s_][1])
            dma("pool", wd[:, 0:G, :], wd_src[c0 * 128:(c0 + G) * 128, :].rearrange("(g p) c -> p g c", p=128), [], [f"fw{s_}2"], s_fw[s_][2])
            for tg in range(4):
                ab = tg % 2
                for j in range(G):
                    pg, pgn = nextF()
                    for k in range(8):
                        mm(pg[:], wg[:, k, j * 128:(j + 1) * 128], fT[:, k, tg * 512:(tg + 1) * 512], k == 0, k == 7, [f"fw{s_}0", "fT"], [pgn])
                    pu, pun = nextF()
                    for k in range(8):
                        mm(pu[:], wu[:, k, j * 128:(j + 1) * 128], fT[:, k, tg * 512:(tg + 1) * 512], k == 0, k == 7, [f"fw{s_}1", "fT"], [pun])
                    act(sg[:, j % 2, :], pg[:], AF.Silu, [pgn], [f"sg{j % 2}"])
                    tt(actT[:, ab, j, :], sg[:, j % 2, :], pu[:], ALU.mult, [f"sg{j % 2}", pun], [f"actT{ab}"])
                for tt_ in range(4):
                    t = tg * 4 + tt_
                    for half in range(2):
                        pd, pdn = nextF()
                        for j in range(G):
                            mm(pd[:], actT[:, ab, j, tt_ * 128:(tt_ + 1) * 128], wd[:, j, half * 512:(half + 1) * 512], j == 0, j == G - 1, [f"actT{ab}", f"fw{s_}2"], [pdn])
                        hs = h[:, t, half * 512:(half + 1) * 512]
                        if gate_col is None:
                            tt(hs, pd[:], hs, ALU.add, [pdn, f"h{t}"], [f"h{t}"])
                        else:
                            stt(hs, pd[:], gates[:, t, gate_col:gate_col + 1], hs, ALU.mult, ALU.add, [pdn, "gates", f"h{t}"], [f"h{t}"])

        def ffn(l):
            if l == 0:
                ffn_norm(l, False)
                for (c0, G) in FF_GROUPS:
                    ffn_group(dwg_d[0], dwu_d[0], dwd_d[0], c0, G, None)
            else:
                ffn_norm(l, True)
                for e_ in range(NE):
                    for (c0, G) in FF_GROUPS:
                        ffn_group(mwg_d[0, e_], mwu_d[0, e_], mwd_d[0, e_], c0, G, e_)

        s_out = [S.dsem(f"out{i}") for i in range(2)]

        def final():
            load_gvec(4)
            for t in range(NT):
                i = t % 2
                ss = small[:, 0:1]
                lnv = small[:, 1:2]
                rstd = small[:, 2:3]
                act(a_bf[:], h[:, t, :], AF.Square, [f"h{t}"], ["a_bf", "ss"], accum=ss)
                act(lnv, ss, AF.Ln, ["ss"], ["lnv"], bias=EPS, scale=1.0 / D)
                act(rstd, lnv, AF.Exp, ["lnv"], ["rstd"], scale=-0.5)
                stt(outst[:, i, :], h[:, t, :], rstd, gvec[:], ALU.mult, ALU.mult, [f"h{t}", "rstd", "gvec"], [f"outst{i}"])
                dma("sp", y_d[t * 128:(t + 1) * 128, :], outst[:, i, :], [f"outst{i}"], [f"y{t}"], s_out[i])
            S.add("sp", None, r=[f"y{t}" for t in range(NT)], w=[])

        def dump_h():
            S.barrier()
            sd = S.dsem("dump")
            for t in range(NT):
                dma("sp", y_d[t * 128:(t + 1) * 128, :], h[:, t, :], [f"h{t}"], [f"y{t}"], sd)
            S.add("sp", None, r=[f"y{t}" for t in range(NT)], w=[])

        def program():
            load_x(xp_d if SI >= 1 else x_d)
            if SI == 0:
                return dump_h()
            mixer_main(0, None)
            save_state(0)
            if stop == 'mixA':
                return dump_h()
            S.barrier()
            ffn(0)
            if stop == 'ffnA':
                return dump_h()
            S.barrier()
            mixer_prepass(1)
            save_state(1)
            if stop == 'preA':
                return dump_h()
            S.barrier()
            load_x(x_d)
            for l in range(2):
                mixer_main(l, l)
                if stop == f'mix{l}':
                    return dump_h()
                S.barrier()
                ffn(l)
                if stop == f'ffn{l}':
                    return dump_h()
                S.barrier()
            final()

        program()
        print("ops:", {e: (len(S.ops[e]), sum(1 for o in S.ops[e] if o.mark)) for e in S.ENGS})
        S.emit(nc, stack)
    return nc


_CACHE = {}


def _prep_shared(inp):
    f = lambda a: np.ascontiguousarray(np.asarray(a, dtype=np.float32))
    consts = np.zeros((128, C_N), np.float32)
    consts[:, C_ID:C_ID + 128] = np.eye(128, dtype=np.float32)
    consts[:, C_ONES:C_ONES + 128] = 1.0 / 256.0
    p = np.arange(128)[:, None] % 64
    i = np.arange(64)[None, :]
    consts[:, C_TRI:C_TRI + 64] = (i >= p).astype(np.float32)
    sm = np.ones((128, 256), np.float32)
    sm[:, 0::64] = 0.0
    consts[:, C_SM:C_SM + 256] = sm
    pvec = np.zeros((2, 128, PV_N), np.float32)
    for l in range(2):
        scw = f(inp["sc_conv_w"][l])
        cfw = f(inp["cf_conv_w"][l])
        for cc in range(2):
            pvec[l, :, PV_SCW + cc * 3:PV_SCW + cc * 3 + 3] = scw[:, cc * 128:(cc + 1) * 128].T
            pvec[l, :, PV_CFW + cc * 31:PV_CFW + cc * 31 + 31] = cfw[:, cc * 128:(cc + 1) * 128].T
            pvec[l, :, PV_CFB + cc] = f(inp["cf_conv_b"][l])[cc * 128:(cc + 1) * 128]
            pvec[l, :, PV_LNG + cc] = f(inp["cf_ln_g"][l])[cc * 128:(cc + 1) * 128]
            pvec[l, :, PV_LNB + cc] = f(inp["cf_ln_b"][l])[cc * 128:(cc + 1) * 128]
            pvec[l, :, PV_BA + cc] = f(inp["gla_b_a"][l])[cc * 128:(cc + 1) * 128]
    bvec = np.zeros((5, 128, D), np.float32)
    bvec[0] = f(inp["attn_norm_g"][0])[None, :]
    bvec[1] = f(inp["attn_norm_g"][1])[None, :]
    bvec[2] = f(inp["ffn_norm_g"][0])[None, :]
    bvec[3] = f(inp["ffn_norm_g"][1])[None, :]
    bvec[4] = f(inp["final_norm_g"])[None, :]
    gng = np.zeros((2, 128, 512), np.float32)
    for l in range(2):
        gng[l] = f(inp["gla_norm_g"][l]).reshape(1, 512)
    shared = {
        "consts": consts, "pvec": pvec, "bvec": bvec, "gng": gng,
        "w_a2": f(inp["gla_w_a2"]), "w_in": f(inp["w_in"]), "w_out": f(inp["w_out"]),
        "dense_w_gate": f(inp["dense_w_gate"]), "dense_w_up": f(inp["dense_w_up"]), "dense_w_down": f(inp["dense_w_down"]),
        "moe_w_router": f(inp["moe_w_router"]), "moe_w_gate": f(inp["moe_w_gate"]), "moe_w_up": f(inp["moe_w_up"]),
        "moe_w_down": f(inp["moe_w_down"]),
    }
    return shared


def kernel(_stop=None, **inputs):
    if _stop is None:
        _stop = DBG_STOP
    x = np.asarray(inputs["x"], dtype=np.float32)
    shared = _prep_shared(inputs)
    if STAGES.index(_stop) < STAGES.index('ffn1'):
        for k_ in ("moe_w_gate", "moe_w_up", "moe_w_down"):
            shared.pop(k_)
    if _stop not in _CACHE:
        _CACHE[_stop] = build_program(_stop)
    nc = _CACHE[_stop]
    in_maps = []
    for r in range(NCORES):
        b, half = r // 2, r % 2
        sel = np.zeros((128, NCORES), np.float32)
        sel[:, 0] = float(half)
        m = dict(shared)
        m["x"] = np.ascontiguousarray(x[b, half * T:(half + 1) * T, :])
        m["x_prev"] = np.ascontiguousarray(x[b, 0:T, :]) if half == 1 else np.zeros((T, D), np.float32)
        m["sel"] = sel
        in_maps.append(m)
    res = run_bass_kernel_spmd(nc, in_maps, core_ids=list(range(NCORES)))
    out = np.zeros((4, 4096, D), np.float32)
    for r in range(NCORES):
        b, half = r // 2, r % 2
        out[b, half * T:(half + 1) * T, :] = np.asarray(res.results[r]["y"], dtype=np.float32)
    return out
```

```python
import contextlib
import os
DBG_EX = os.environ.get('DBG_EX', '')
DBG_MB = int(os.environ.get('DBG_MB', '99'))
DBG_G = os.environ.get('DBG_G', '')
DBG_H = os.environ.get('DBG_H', '')
DBG_Y = os.environ.get('DBG_Y', '')
DBG_STOP = os.environ.get('DBG_STOP', 'final')
import numpy as np
import concourse.bass as bass
import concourse.mybir as mybir
from concourse.bass_utils import run_bass_kernel_spmd

F32 = mybir.dt.float32
BF16 = mybir.dt.bfloat16
AF = mybir.ActivationFunctionType
ALU = mybir.AluOpType
AX = mybir.AxisListType

NCORES = 8
D = 1024
T = 2048
NT = 16
TB = 256
NBLK = T // TB
HALO = 32
EXT = HALO + TB
D_IN = 2832
D_FF = 2816
NE = 8
EPS = 1e-6
O_SCB, O_SCC, O_SCV, O_CFA, O_CFG, O_Q, O_K, O_V, O_ALR, O_R = 0, 256, 512, 768, 1024, 1280, 1536, 1792, 2304, 2320
FF_GROUPS = [(0, 4), (4, 4), (8, 4), (12, 4), (16, 4), (20, 2)]
PV_SCW, PV_CFW, PV_CFB, PV_LNG, PV_LNB, PV_BA, PV_N = 0, 6, 68, 70, 72, 74, 76
C_ID, C_ONES, C_TRI, C_SM, C_N = 0, 128, 256, 320, 576


class DSem:
    def __init__(self, name):
        self.name = name
        self.count = 0
        self.handle = None


class Op:
    __slots__ = ("eng", "fn", "waits", "mark", "semval", "idx", "dma")

    def __init__(self, eng, fn, dma):
        self.eng, self.fn, self.dma = eng, fn, dma
        self.waits = []
        self.mark = False
        self.semval = None
        self.idx = 0


class Sched:
    ENGS = ["pe", "act", "dve", "pool", "sp"]

    def __init__(self):
        self.ops = {e: [] for e in self.ENGS}
        self.res = {}
        self.pending = {e: [] for e in self.ENGS}
        self.dsems = []

    def dsem(self, name):
        s = DSem(name)
        self.dsems.append(s)
        return s

    def add(self, eng, fn, r=(), w=(), dma=None, rg=None):
        op = Op(eng, fn, dma)
        op.idx = len(self.ops[eng])
        deps = []
        force = None
        if eng == "pe":
            prev = getattr(self, "_pe_rg", None)
            if rg is not None and prev is not None and (rg[0] + rg[1] <= prev[0] or prev[0] + prev[1] <= rg[0]):
                force = self.ops["pe"][-1]
            self._pe_rg = rg
        for x in r:
            st = self.res.get(x)
            if st is not None and st[0] is not None:
                deps.append(st[0])
        for x in w:
            st = self.res.get(x)
            if st is not None:
                if st[0] is not None:
                    deps.append(st[0])
                deps.extend(st[1])
        deps.extend(self.pending[eng])
        self.pending[eng] = []
        if force is not None:
            force.mark = True
            op.waits.append(("op", force))
        seen = set()
        for tok in deps:
            if id(tok) in seen:
                continue
            seen.add(id(tok))
            if tok[0] == "op":
                p = tok[1]
                if p.eng == eng:
                    if eng == "pe":
                        continue
                    if dma is None and (op.idx - p.idx) > 2:
                        continue
                p.mark = True
            op.waits.append(tok)
        if dma is not None:
            dma.count += 1
            tok = ("dma", dma, dma.count * 16)
        else:
            tok = ("op", op)
        for x in r:
            st = self.res.setdefault(x, [None, []])
            st[1].append(tok)
        for x in w:
            self.res[x] = [tok, []]
        self.ops[eng].append(op)
        return op

    def barrier(self):
        toks = []
        for e in self.ENGS:
            if self.ops[e]:
                last = None
                for o in reversed(self.ops[e]):
                    if o.dma is None and o.fn is not None:
                        last = o
                        break
                if last is not None:
                    last.mark = True
                    toks.append(("op", last))
        for s in self.dsems:
            if s.count:
                toks.append(("dma", s, s.count * 16))
        for e in self.ENGS:
            self.pending[e] = [t for t in toks if not (t[0] == "op" and t[1].eng == e and e == "pe")]
        self.res = {}

    def emit(self, nc, stack):
        EPOCH = 30000
        engsems = {}
        for e in self.ENGS:
            n = 0
            for o in self.ops[e]:
                if o.mark:
                    n += 1
                    o.semval = n
            nep = max(1, (n + EPOCH - 1) // EPOCH)
            engsems[e] = [stack.enter_context(nc.semaphore(f"s_{e}{i}")) for i in range(nep)]
        for s in self.dsems:
            if s.count:
                s.handle = stack.enter_context(nc.semaphore("d_" + s.name))

        def semof(e, val):
            i = (val - 1) // EPOCH
            return engsems[e][i], val - i * EPOCH

        block = stack.enter_context(nc.Block())

        def run(ename):
            def body(eng):
                waited = {}
                for o in self.ops[ename]:
                    for tok in o.waits:
                        if tok[0] == "op":
                            sem, val = semof(tok[1].eng, tok[1].semval)
                        else:
                            sem, val = tok[1].handle, tok[2]
                        key = id(sem)
                        if waited.get(key, 0) >= val:
                            continue
                        waited[key] = val
                        eng.wait_ge(sem, val)
                    if o.fn is None:
                        continue
                    ins = o.fn(eng)
                    if o.dma is not None:
                        ins.then_inc(o.dma.handle, 16)
                    elif o.mark:
                        sem, _ = semof(ename, o.semval)
                        ins.then_inc(sem, 1)
            return body

        block.tensor(run("pe"))
        block.scalar(run("act"))
        block.vector(run("dve"))
        block.gpsimd(run("pool"))
        block.sync(run("sp"))


STAGES = ['load', 'mixA', 'ffnA', 'preA', 'mix0', 'ffn0', 'mix1', 'ffn1', 'final']


def build_program(stop='final'):
    nc = bass.Bass("TRN2", target_bir_lowering=False)
    S = Sched()
    SI = STAGES.index(stop)
    need_moe = SI >= STAGES.index('ffn1')

    def din(name, shape):
        return nc.dram_tensor(name, list(shape), F32, kind="ExternalInput").ap()

    x_d = din("x", [T, D])
    xp_d = din("x_prev", [T, D])
    sel_d = din("sel", [128, NCORES])
    consts_d = din("consts", [128, C_N])
    pvec_d = din("pvec", [2, 128, PV_N])
    bvec_d = din("bvec", [5, 128, D])
    gng_d = din("gng", [2, 128, 512])
    wa2_d = din("w_a2", [2, 16, 256])
    win_d = din("w_in", [2, D, D_IN])
    wout_d = din("w_out", [2, D, D])
    dwg_d = din("dense_w_gate", [1, D, D_FF])
    dwu_d = din("dense_w_up", [1, D, D_FF])
    dwd_d = din("dense_w_down", [1, D_FF, D])
    wr_d = din("moe_w_router", [1, D, NE])
    if need_moe:
        mwg_d = din("moe_w_gate", [1, NE, D, D_FF])
        mwu_d = din("moe_w_up", [1, NE, D, D_FF])
        mwd_d = din("moe_w_down", [1, NE, D_FF, D])
    y_d = nc.dram_tensor("y", [T, D], F32, kind="ExternalOutput").ap()

    stack = contextlib.ExitStack()
    with stack:
        def sb(name, shape, dt=F32):
            return stack.enter_context(nc.sbuf_tensor("sb_" + name, list(shape), dt))

        h = sb("h", [128, NT, D])
        wbuf = sb("wbuf", [128, 8 * D_IN + 8 * D], BF16)
        W_in = wbuf[:, 0:8 * D_IN].rearrange("p (k c) -> p k c", k=8)
        W_out = wbuf[:, 8 * D_IN:8 * D_IN + 8 * D].rearrange("p (k c) -> p k c", k=8)
        FW = []
        for s_ in range(2):
            base = s_ * 12288
            wg = wbuf[:, base:base + 4096].rearrange("p (k c) -> p k c", k=8)
            wu = wbuf[:, base + 4096:base + 8192].rearrange("p (k c) -> p k c", k=8)
            wd = wbuf[:, base + 8192:base + 12288].rearrange("p (g c) -> p g c", g=4)
            FW.append((wg, wu, wd))
        RR_N = 10240
        RR = sb("RR", [128, RR_N], F32)
        fT = RR[:, 0:8192].bitcast(BF16).rearrange("p (k t) -> p k t", k=8)
        _off = [0]

        def carve(n_f32, dt=F32, shape=None):
            a = RR[:, _off[0]:_off[0] + n_f32]
            _off[0] += n_f32
            if dt == BF16:
                a = a.bitcast(BF16)
            return a

        S_A = carve(256).rearrange("p (a t) -> p a t", a=2)
        S_T = carve(256).rearrange("p (a t) -> p a t", a=2)
        S_bf = carve(128, BF16).rearrange("p (a t) -> p a t", a=2)
        _x0 = _off[0]
        csp = carve(512).rearrange("p (a t) -> p a t", a=2)
        E_ = carve(512).rearrange("p (a t) -> p a t", a=2)
        Ei = carve(512).rearrange("p (a t) -> p a t", a=2)
        qtT = carve(256, BF16).rearrange("p (a t) -> p a t", a=2)
        ktT = carve(256, BF16).rearrange("p (a t) -> p a t", a=2)
        kt = carve(256, BF16).rearrange("p (a t) -> p a t", a=2)
        vv = carve(512, BF16).rearrange("p (a t) -> p a t", a=2)
        sr = carve(512, BF16).rearrange("p (a t) -> p a t", a=2)
        m_ = carve(2 * EXT).rearrange("p (a t) -> p a t", a=2)
        u_ = carve(EXT, BF16).rearrange("p (a t) -> p a t", a=2)
        tmpx = carve(EXT)
        acc_sc = carve(256)
        cfc = carve(512).rearrange("p (a t) -> p a t", a=2)
        cfsq = carve(512).rearrange("p (a t) -> p a t", a=2)
        mean_sb = carve(256)
        var_sb = carve(256)
        rstd_sb = carve(256)
        yT = carve(1024, BF16).rearrange("p (a t) -> p a t", a=8)
        o_sb = carve(512)
        og = carve(512)
        ogb = carve(256, BF16)
        AT = carve(128, BF16).rearrange("p (a t) -> p a t", a=4)
        alrT = carve(256)
        assert _off[0] <= RR_N, _off[0]
        _off[0] = _x0
        stS = carve(256)
        stH = carve(1024)
        haloh = carve(1024)
        outst = RR[:, 0:2048].rearrange("p (a t) -> p a t", a=2)

        aTx = sb("aTx", [128, 8, EXT], BF16)
        FR = sb("FR", [128, 4608], F32)
        actT = FR[:, 0:2048].bitcast(BF16).rearrange("p (a j t) -> p a j t", a=2, j=4)
        f32t = FR[:, 2048:3072]
        f32T = FR[:, 3072:4096].rearrange("p (k t) -> p k t", k=8)
        sg = FR[:, 4096:4608].bitcast(BF16).rearrange("p (a t) -> p a t", a=2)
        D_cf = FR[:, 0:3968].bitcast(BF16).rearrange("p (j c) -> p j c", j=62)
        aTx1 = FR[:, 0:1152].bitcast(BF16).rearrange("p (k t) -> p k t", k=8)
        csp1 = FR[:, 1152:1664].rearrange("p (a t) -> p a t", a=2)
        E_1 = FR[:, 1664:2176].rearrange("p (a t) -> p a t", a=2)
        Ei1 = FR[:, 2176:2688].rearrange("p (a t) -> p a t", a=2)
        a_bf = sb("a_bf", [128, D], BF16)
        gvec = sb("gvec", [128, D])
        consts = sb("consts", [128, C_N])
        ident_bf = sb("ident_bf", [128, 128], BF16)
        pvec = sb("pvec", [128, 2, PV_N])
        nba = sb("nba", [128, 2, 2])
        gng = sb("gng", [128, 2, 512])
        wa2 = sb("wa2", [16, 2, 256])
        wr = sb("wr", [128, 8, NE])
        sel = sb("sel", [128, NCORES])
        small = sb("small", [128, 64])
        Ssave = sb("Ssave", [128, 2, 256])
        Hsave = sb("Hsave", [128, 2, 8 * HALO], BF16)
        gates = sb("gates", [128, NT, NE])

        psF = [stack.enter_context(nc.psum_tensor(f"psF{i}", [128, 512], F32)) for i in range(6)]
        psB = [stack.enter_context(nc.psum_tensor(f"psB{i}", [128, 1024], BF16)) for i in range(2)]
        _rr = [0, 0]

        def nextF():
            i = _rr[0] % 5
            _rr[0] += 1
            return psF[i], f"F{i}"

        def nextB():
            i = _rr[1] % 2
            _rr[1] += 1
            return psB[i], f"B{i}"

        ident = consts[:, C_ID:C_ID + 128]
        ones256 = consts[:, C_ONES:C_ONES + 128]
        tri = consts[:, C_TRI:C_TRI + 64]
        smask = consts[:, C_SM:C_SM + 256]

        def mm(out, lhsT, rhs, start, stop, r, w):
            rg = (lhsT.base_partition(), lhsT.partition_size())
            S.add("pe", lambda e: e.matmul(out, lhsT=lhsT, rhs=rhs, start=start, stop=stop), r=r, w=w, rg=rg)

        def tr(out, in_, idn, r, w):
            rg = (in_.base_partition(), in_.partition_size())
            S.add("pe", lambda e: e.transpose(out=out, in_=in_, identity=idn), r=r, w=w, rg=rg)

        def act(out, in_, func, r, w, bias=None, scale=None, accum=None):
            kw = {}
            if bias is not None:
                kw["bias"] = bias
            if scale is not None:
                kw["scale"] = scale
            if accum is not None:
                kw["accum_out"] = accum
            S.add("act", lambda e: e.activation(out=out, in_=in_, func=func, **kw), r=r, w=w)

        def tt(out, in0, in1, op, r, w, eng="dve"):
            S.add(eng, lambda e: e.tensor_tensor(out=out, in0=in0, in1=in1, op=op), r=r, w=w)

        def ts(out, in0, s1, op0, r, w, s2=None, op1=None, eng="dve"):
            if op1 is None:
                S.add(eng, lambda e: e.tensor_scalar(out=out, in0=in0, scalar1=s1, scalar2=None, op0=op0), r=r, w=w)
            else:
                S.add(eng, lambda e: e.tensor_scalar(out=out, in0=in0, scalar1=s1, scalar2=s2, op0=op0, op1=op1), r=r, w=w)

        def stt(out, in0, scalar, in1, op0, op1, r, w):
            S.add("dve", lambda e: e.scalar_tensor_tensor(out=out, in0=in0, scalar=scalar, in1=in1, op0=op0, op1=op1), r=r, w=w)

        def cp(out, in_, r, w, eng="dve"):
            S.add(eng, lambda e: e.tensor_copy(out=out, in_=in_), r=r, w=w)

        def dma(q, out, in_, r, w, sem):
            S.add(q, lambda e: e.dma_start(out=out, in_=in_), r=r, w=w, dma=sem)

        s_misc = [S.dsem(f"misc{i}") for i in range(10)]
        dma("sp", consts[:], consts_d, [], ["consts"], s_misc[0])
        dma("pool", ident_bf[:], consts_d[:, C_ID:C_ID + 128], [], ["ident_bf"], s_misc[1])
        dma("sp", pvec[:], pvec_d.rearrange("l p n -> p l n"), [], ["pvec"], s_misc[2])
        dma("sp", gng[:], gng_d.rearrange("l p n -> p l n"), [], ["gng"], s_misc[3])
        dma("sp", wa2[:], wa2_d.rearrange("l r n -> r l n"), [], ["wa2"], s_misc[4])
        dma("sp", wr[:], wr_d[0].rearrange("(k p) e -> p k e", p=128), [], ["wr"], s_misc[5])
        dma("sp", sel[:], sel_d, [], ["sel"], s_misc[6])
        s_x = [S.dsem(f"x{t}") for t in range(NT)]

        def load_x(src):
            for t in range(NT):
                dma("sp", h[:, t, :], src[t * 128:(t + 1) * 128, :], [], [f"h{t}"], s_x[t])
        ts(nba[:], pvec[:, :, PV_BA:PV_BA + 2], -1.0, ALU.mult, ["pvec"], ["nba"])

        s_win = [S.dsem(f"win{k}") for k in range(8)]
        s_wout = S.dsem("wout")
        s_gv = S.dsem("gv")
        s_fw = [[S.dsem(f"fw{s_}{j}") for j in range(3)] for s_ in range(2)]

        def load_mixer_weights(l):
            for k in range(8):
                dma("pool", W_in[:, k, :], win_d[l, k * 128:(k + 1) * 128, :], [], [f"Win{k}"] + [f"fw{s_}{j}" for s_ in range(2) for j in range(3)], s_win[k])
            dma("pool", W_out[:], wout_d[l].rearrange("(k p) c -> p k c", p=128), [], ["Wout"], s_wout)

        def norm_tile(src, src_res, nrows, gidx_res, dstT, dst_res, want_f32=False):
            ss = small[0:nrows, 0:1]
            lnv = small[0:nrows, 1:2]
            rstd = small[0:nrows, 2:3]
            act(a_bf[0:nrows, :], src, AF.Square, [src_res], ["a_bf", "ss"], accum=ss)
            act(lnv, ss, AF.Ln, ["ss"], ["lnv"], bias=EPS, scale=1.0 / D)
            act(rstd, lnv, AF.Exp, ["lnv"], ["rstd"], scale=-0.5)
            if want_f32:
                stt(f32t[0:nrows, :], src, rstd, gvec[0:nrows, :], ALU.mult, ALU.mult, [src_res, "rstd", gidx_res], ["f32t"])
                act(a_bf[0:nrows, :], f32t[0:nrows, :], AF.Copy, ["f32t"], ["a_bf"])
            else:
                stt(a_bf[0:nrows, :], src, rstd, gvec[0:nrows, :], ALU.mult, ALU.mult, [src_res, "rstd", gidx_res], ["a_bf"])
            pb, pbn = nextB()
            for k in range(8):
                tr(pb[:, k * nrows:(k + 1) * nrows], a_bf[0:nrows, k * 128:(k + 1) * 128], ident_bf[0:nrows, 0:nrows],
                   ["a_bf", "ident_bf"], [pbn])
            cp(dstT, pb[:, 0:8 * nrows].rearrange("p (k t) -> p k t", k=8), [pbn], [dst_res], eng="act" if False else "dve")

        def load_gvec(i):
            dma("sp", gvec[:], bvec_d[i], [], ["gvec"], s_gv)

        class BS:
            pass

        PB = []
        for i_, (a_, c_, e_, ei_) in enumerate([(aTx, csp, E_, Ei), (aTx, csp, E_, Ei)]):
            o_ = BS()
            o_.aTx, o_.csp, o_.E, o_.Ei = a_, c_, e_, ei_
            o_.n_aTx, o_.n_csp, o_.n_E, o_.n_Ei = "aTx0", "csp0", "E0", "Ei0"
            PB.append(o_)

        def proj_fm(col0, ncols, rhs, rhs_res, N):
            p, pn = nextF()
            for k in range(8):
                mm(p[0:ncols, 0:N], W_in[:, k, col0:col0 + ncols], rhs[:, k, :], k == 0, k == 7, [f"Win{k}", rhs_res], [pn])
            return p, pn

        def prep_stream(l, b, P, Pprev):
            if Pprev is not None:
                cp(P.aTx[:, :, 0:HALO], Pprev.aTx[:, :, EXT - HALO:EXT], [Pprev.n_aTx], [P.n_aTx])
            for ti in range(2):
                t = b * 2 + ti
                norm_tile(h[:, t, :], f"h{t}", 128, "gvec", P.aTx[:, :, HALO + ti * 128:HALO + (ti + 1) * 128], P.n_aTx)
                yield
            rhs = P.aTx[:, :, HALO:EXT]
            p, pn = proj_fm(O_ALR, 16, rhs, P.n_aTx, TB)
            cp(alrT[0:16, :], p[0:16, 0:TB], [pn], ["alrT"])
            yield
            pz, pzn = nextF()
            for hp in range(2):
                mm(pz[:, hp * 256:(hp + 1) * 256], wa2[0:16, l, hp * 128:(hp + 1) * 128], alrT[0:16, :], True, True, ["wa2", "alrT"], [pzn])
            for hp in range(2):
                act(P.csp[:, hp, :], pz[:, hp * 256:(hp + 1) * 256], AF.Exp, [pzn, "nba"], [P.n_csp], bias=nba[:, l, hp:hp + 1], scale=-1.0)
            yield
            act(P.csp[:], P.csp[:], AF.Ln, [P.n_csp], [P.n_csp], bias=1.0)
            for hp in range(2):
                S.add("dve", (lambda hp_: (lambda e: e.tensor_tensor_scan(out=P.csp[:, hp_, :], data0=smask, data1=P.csp[:, hp_, :], initial=0.0, op0=ALU.mult, op1=ALU.add)))(hp),
                      r=[P.n_csp, "consts"], w=[P.n_csp])
            yield
            act(P.Ei[:], P.csp[:], AF.Exp, [P.n_csp], [P.n_Ei], scale=1.0 / 16.0)
            act(P.E[:], P.csp[:], AF.Exp, [P.n_csp], [P.n_E], scale=-1.0 / 16.0)
            yield

        def kv_block(P):
            rhs = P.aTx[:, :, HALO:EXT]
            pk, pkn = nextF()
            for hp in range(2):
                for k in range(8):
                    mm(pk[:, hp * 256:(hp + 1) * 256], W_in[:, k, O_K + hp * 128:O_K + (hp + 1) * 128], rhs[:, k, :], k == 0, k == 7, [f"Win{k}", P.n_aTx], [pkn])
            tt(ktT[:], pk[:].rearrange("p (a t) -> p a t", a=2), P.Ei[:], ALU.mult, [pkn, P.n_Ei], ["ktT"])
            pb, pbn = nextB()
            for ti in range(2):
                for hp in range(2):
                    tr(pb[:, ti * 256 + hp * 128: ti * 256 + (hp + 1) * 128], ktT[:, hp, ti * 128:(ti + 1) * 128], ident_bf[:], ["ktT", "ident_bf"], [pbn])
            cp(kt[:], pb[:, 0:512].rearrange("p (a t) -> p a t", a=2), [pbn], ["kt"])
            for ti in range(2):
                pv, pvn = nextF()
                for k in range(8):
                    mm(pv[:], P.aTx[:, k, HALO + ti * 128:HALO + (ti + 1) * 128], W_in[:, k, O_V:O_V + 512], k == 0, k == 7, [f"Win{k}", P.n_aTx], [pvn])
                act(vv[:, ti, :], pv[:], AF.Copy, [pvn], ["vv"])

        def state_update(c, P):
            ti, par = c // 2, c % 2
            rows = slice(par * 64, par * 64 + 64)
            pS, pSn = nextF()
            for hh in range(4):
                hp, hl = hh // 2, hh % 2
                mm(pS[hl * 64:(hl + 1) * 64, hp * 128:(hp + 1) * 128], kt[rows, ti, hh * 64:(hh + 1) * 64], vv[rows, ti, hh * 128:(hh + 1) * 128],
                   True, True, ["kt", "vv"], [pSn])
            tt(S_T[:], pS[:, 0:256].rearrange("p (a t) -> p a t", a=2), S_A[:], ALU.add, [pSn, "S_A"], ["S_T"])
            for hp in range(2):
                ts(S_A[:, hp, :], S_T[:, hp, :], P.E[:, hp, c * 64 + 63:c * 64 + 64], ALU.mult, ["S_T", P.n_E], ["S_A"])
            cp(S_bf[:], S_A[:], ["S_A"], ["S_bf"])

        def prepass_stream(P):
            kv_block(P)
            yield
            for c in range(4):
                state_update(c, P)
                yield

        def interleave(*gens):
            gens = [g_ for g_ in gens if g_ is not None]
            while gens:
                for g_ in list(gens):
                    try:
                        next(g_)
                    except StopIteration:
                        gens.remove(g_)

        def conv_stream(l, b, P):
            aX, nX = P.aTx, P.n_aTx
            rhs = aX[:, :, HALO:EXT]
            for cc in range(2):
                pc, pcn = nextF()
                for k in range(8):
                    mm(pc[:, 0:EXT], W_in[:, k, O_SCC + cc * 128:O_SCC + (cc + 1) * 128], aX[:, k, :], k == 0, k == 7, [f"Win{k}", nX], [pcn])
                pv, pvn = nextF()
                for k in range(8):
                    mm(pv[:, 0:EXT], W_in[:, k, O_SCV + cc * 128:O_SCV + (cc + 1) * 128], aX[:, k, :], k == 0, k == 7, [f"Win{k}", nX], [pvn])
                act(tmpx[:], pc[:, 0:EXT], AF.Copy, [pcn], ["tmpx"])
                tt(m_[:, cc, :], tmpx[:], pv[:, 0:EXT], ALU.mult, ["tmpx", pvn], ["m"])
                yield
                w0 = pvec[:, l, PV_SCW + cc * 3 + 0:PV_SCW + cc * 3 + 1]
                w1 = pvec[:, l, PV_SCW + cc * 3 + 1:PV_SCW + cc * 3 + 2]
                w2 = pvec[:, l, PV_SCW + cc * 3 + 2:PV_SCW + cc * 3 + 3]
                ts(acc_sc[:], m_[:, cc, HALO - 2:HALO - 2 + TB], w0, ALU.mult, ["m", "pvec"], ["acc_sc"])
                stt(acc_sc[:], m_[:, cc, HALO - 1:HALO - 1 + TB], w1, acc_sc[:], ALU.mult, ALU.add, ["m", "pvec", "acc_sc"], ["acc_sc"])
                stt(acc_sc[:], m_[:, cc, HALO:HALO + TB], w2, acc_sc[:], ALU.mult, ALU.add, ["m", "pvec", "acc_sc"], ["acc_sc"])
                pb_, pbn_ = proj_fm(O_SCB + cc * 128, 128, rhs, nX, TB)
                tt(yT[:, cc, :], acc_sc[:], pb_[:, 0:TB], ALU.mult, ["acc_sc", pbn_], ["yT_sc"])
                yield
            for cc in range(2):
                pa, pan = nextF()
                for k in range(8):
                    mm(pa[:, 0:EXT], W_in[:, k, O_CFA + cc * 128:O_CFA + (cc + 1) * 128], aX[:, k, :], k == 0, k == 7, [f"Win{k}", nX], [pan])
                pg, pgn = nextF()
                for k in range(8):
                    mm(pg[:, 0:EXT], W_in[:, k, O_CFG + cc * 128:O_CFG + (cc + 1) * 128], aX[:, k, :], k == 0, k == 7, [f"Win{k}", nX], [pgn])
                act(tmpx[:], pg[:, 0:EXT], AF.Tanh, [pgn], ["tmpx"], scale=0.5)
                stt(u_[:, cc, :], tmpx[:], 1.0, pa[:, 0:EXT], ALU.add, ALU.mult, ["tmpx", pan], ["u"])
                yield
                pcv, pcvn = nextF()
                for kk in range(31):
                    mm(pcv[:, 0:TB], D_cf[:, cc * 31 + kk, :], u_[:, cc, HALO - 30 + kk:HALO - 30 + kk + TB], kk == 0, kk == 30, ["Dcf", "u"], [pcvn])
                act(cfc[:, cc, :], pcv[:, 0:TB], AF.Identity, [pcvn, "pvec"], ["cfc"], bias=pvec[:, l, PV_CFB + cc:PV_CFB + cc + 1], scale=0.5)
                act(cfsq[:, cc, :], cfc[:, cc, :], AF.Square, ["cfc"], ["cfsq"])
                yield
            pm, pmn = nextF()
            for cc in range(2):
                mm(pm[:, 0:TB], ones256, cfc[:, cc, :], cc == 0, cc == 1, ["consts", "cfc"], [pmn])
            for cc in range(2):
                mm(pm[:, TB:2 * TB], ones256, cfsq[:, cc, :], cc == 0, cc == 1, ["consts", "cfsq"], [pmn])
            cp(mean_sb[:], pm[:, 0:TB], [pmn], ["mean"])
            tt(var_sb[:], mean_sb[:], mean_sb[:], ALU.mult, ["mean"], ["var"])
            tt(var_sb[:], pm[:, TB:2 * TB], var_sb[:], ALU.subtract, [pmn, "var"], ["var"])
            yield
            act(rstd_sb[:], var_sb[:], AF.Ln, ["var"], ["rstdc"], bias=EPS)
            act(rstd_sb[:], rstd_sb[:], AF.Exp, ["rstdc"], ["rstdc"], scale=-0.5)
            for cc in range(2):
                tt(cfc[:, cc, :], cfc[:, cc, :], mean_sb[:], ALU.subtract, ["cfc", "mean"], ["cfc"])
                tt(cfc[:, cc, :], cfc[:, cc, :], rstd_sb[:], ALU.mult, ["cfc", "rstdc"], ["cfc"])
                act(yT[:, 2 + cc, :], cfc[:, cc, :], AF.Silu, ["cfc", "pvec"], ["yT_cf"],
                    bias=pvec[:, l, PV_LNB + cc:PV_LNB + cc + 1], scale=pvec[:, l, PV_LNG + cc:PV_LNG + cc + 1])
                yield

        def gla_stream(l, b, P):
            aX, nX = P.aTx, P.n_aTx
            rhs = aX[:, :, HALO:EXT]
            pq, pqn = nextF()
            for hp in range(2):
                for k in range(8):
                    mm(pq[:, hp * 256:(hp + 1) * 256], W_in[:, k, O_Q + hp * 128:O_Q + (hp + 1) * 128], rhs[:, k, :], k == 0, k == 7, [f"Win{k}", nX], [pqn])
            stt(qtT[:], pq[:].rearrange("p (a t) -> p a t", a=2), 0.125, P.E[:], ALU.mult, ALU.mult, [pqn, P.n_E], ["qtT"])
            yield
            kv_block(P)
            yield
            for ti in range(2):
                pr, prn = nextF()
                for k in range(8):
                    mm(pr[:], aX[:, k, HALO + ti * 128:HALO + (ti + 1) * 128], W_in[:, k, O_R:O_R + 512], k == 0, k == 7, [f"Win{k}", nX], [prn])
                act(sr[:, ti, :], pr[:], AF.Silu, [prn], ["sr"])
                yield
            po, pon = psF[5], "F5"
            for ti in range(2):
                for par in range(2):
                    c = ti * 2 + par
                    rows = slice(par * 64, par * 64 + 64)
                    cols = slice(c * 64, c * 64 + 64)
                    pA, pAn = nextF()
                    for hh in (0, 2, 1, 3):
                        hp, hl = hh // 2, hh % 2
                        fr = slice(hl * 64, hl * 64 + 64)
                        mm(pA[rows, hh * 64:(hh + 1) * 64], ktT[fr, hp, cols], qtT[fr, hp, cols], True, True, ["ktT", "qtT"], [pAn])
                    tt(AT[rows, :, :], pA[rows, 0:256].rearrange("p (a t) -> p a t", a=4), tri[rows, :].unsqueeze(1).to_broadcast([64, 4, 64]),
                       ALU.mult, [pAn, "consts"], ["AT"])
                    yield
                    for hh in ((0, 2, 1, 3) if par == 0 else (1, 3, 0, 2)):
                        hp, hl = hh // 2, hh % 2
                        fr = slice(hl * 64, hl * 64 + 64)
                        mm(po[rows, hh * 128:(hh + 1) * 128], qtT[fr, hp, cols], S_bf[fr, hp, :], True, False, ["qtT", "S_bf"], [pon])
                        mm(po[rows, hh * 128:(hh + 1) * 128], AT[rows, hh, :], vv[rows, ti, hh * 128:(hh + 1) * 128], False, True, ["AT", "vv"], [pon])
                    yield
                    state_update(c, P)
                    yield
                act(o_sb[:], po[:], AF.Copy, [pon], ["o_sb"])
                for hh in range(4):
                    act(og[:, hh * 128:(hh + 1) * 128], o_sb[:, hh * 128:(hh + 1) * 128], AF.Square, ["o_sb"], ["og", "ss4"], accum=small[:, 8 + hh:9 + hh])
                act(small[:, 12:16], small[:, 8:12], AF.Ln, ["ss4"], ["ln4"], bias=EPS, scale=1.0 / 128.0)
                act(small[:, 16:20], small[:, 12:16], AF.Exp, ["ln4"], ["rstd4"], scale=-0.5)
                yield
                for hh in range(4):
                    stt(og[:, hh * 128:(hh + 1) * 128], o_sb[:, hh * 128:(hh + 1) * 128], small[:, 16 + hh:17 + hh], gng[:, l, hh * 128:(hh + 1) * 128],
                        ALU.mult, ALU.mult, ["o_sb", "rstd4", "gng"], ["og"])
                tt(ogb[:], og[:], sr[:, ti, :], ALU.mult, ["og", "sr"], ["ogb"])
                pb, pbn = nextB()
                for j in range(4):
                    tr(pb[:, j * 128:(j + 1) * 128], ogb[:, j * 128:(j + 1) * 128], ident_bf[:], ["ogb", "ident_bf"], [pbn])
                cp(yT[:, 4:8, ti * 128:(ti + 1) * 128], pb[:, 0:512].rearrange("p (a t) -> p a t", a=4), [pbn], ["yT_gla"])
                yield

        def wout_block(b):
            for ti in range(2):
                t = b * 2 + ti
                for half in range(2):
                    pw, pwn = nextF()
                    for k in range(8):
                        mm(pw[:], yT[:, k, ti * 128:(ti + 1) * 128], W_out[:, k, half * 512:(half + 1) * 512], k == 0, k == 7,
                           ["yT_sc", "yT_cf", "yT_gla", "Wout"], [pwn])
                    tt(h[:, t, half * 512:(half + 1) * 512], pw[:], h[:, t, half * 512:(half + 1) * 512], ALU.add, [pwn, f"h{t}"], [f"h{t}"])

        def save_state(i):
            P = PB[(NBLK - 1) % 2]
            ts(Ssave[:, i, :], S_A[:].rearrange("p a t -> p (a t)"), sel[:, 0:1], ALU.mult, ["S_A", "sel"], [f"Ssave{i}"])
            ts(Hsave[:, i, :].rearrange("p (k t) -> p k t", k=8), P.aTx[:, :, EXT - HALO:EXT], sel[:, 0:1], ALU.mult, [P.n_aTx, "sel"], [f"Hsave{i}"])

        def mixer_main(l, src):
            load_gvec(l)
            load_mixer_weights(l)
            if src is None:
                S.add("dve", lambda e: e.memset(S_A[:], 0.0), r=[], w=["S_A"])
                S.add("dve", lambda e: e.memset(PB[0].aTx[:, :, 0:HALO], 0.0), r=[], w=[PB[0].n_aTx])
            else:
                cp(S_A[:].rearrange("p a t -> p (a t)"), Ssave[:, src, :], [f"Ssave{src}"], ["S_A"])
                cp(PB[0].aTx[:, :, 0:HALO], Hsave[:, src, :].rearrange("p (k t) -> p k t", k=8), [f"Hsave{src}"], [PB[0].n_aTx])
            cp(S_bf[:], S_A[:], ["S_A"], ["S_bf"])
            for j in range(62):
                ts(D_cf[:, j, :], ident_bf[:], pvec[:, l, PV_CFW + j:PV_CFW + j + 1], ALU.mult, ["ident_bf", "pvec"], ["Dcf"])
            P = PB[0]
            for b in range(NBLK):
                interleave(prep_stream(l, b, P, P if b > 0 else None))
                interleave(gla_stream(l, b, P), conv_stream(l, b, P))
                wout_block(b)

        def mixer_prepass(l):
            load_gvec(l)
            load_mixer_weights(l)
            S.add("dve", lambda e: e.memset(S_A[:], 0.0), r=[], w=["S_A"])
            P = PB[0]
            for b in range(NBLK):
                interleave(prep_stream(l, b, P, P if b > 0 else None))
                interleave(prepass_stream(P))

        def ffn_norm_tiles(moe, tiles):
            for t in tiles:
                norm_tile(h[:, t, :], f"h{t}", 128, "gvec", fT[:, :, t * 128:(t + 1) * 128], f"fT{t // 4}", want_f32=moe)
                if moe:
                    router_tile(t)

        def router_tile(t):
            pl, pln = nextF()
            for half in range(2):
                pt, ptn = nextF()
                for j in range(4):
                    k = half * 4 + j
                    tr(pt[:, j * 128:(j + 1) * 128], f32t[:, k * 128:(k + 1) * 128], ident, ["f32t", "consts"], [ptn])
                cp(f32T[:, half * 4:(half + 1) * 4, :], pt[:].rearrange("p (a t) -> p a t", a=4), [ptn], ["f32T"])
            for k in range(8):
                mm(pl[:, 0:NE], f32T[:, k, :], wr[:, k, :], k == 0, k == 7, ["f32T", "wr"], [pln])
            lg = small[:, 24:32]
            m1 = small[:, 32:33]
            m2 = small[:, 33:34]
            eq = small[:, 34:42]
            l2 = small[:, 42:50]
            ex = small[:, 50:58]
            den = small[:, 58:59]
            nm1 = small[:, 59:60]
            cp(lg, pl[:, 0:NE], [pln], ["lg"])
            S.add("dve", lambda e: e.reduce_max(out=m1, in_=lg, axis=AX.X), r=["lg"], w=["m1"])
            ts(eq, lg, m1, ALU.is_equal, ["lg", "m1"], ["eq"])
            stt(l2, eq, -1e30, lg, ALU.mult, ALU.add, ["eq", "lg"], ["l2"])
            S.add("dve", lambda e: e.reduce_max(out=m2, in_=l2, axis=AX.X), r=["l2"], w=["m2"])
            ts(eq, lg, m2, ALU.is_ge, ["lg", "m2"], ["eq"])
            ts(nm1, m1, -1.0, ALU.mult, ["m1"], ["nm1"])
            act(ex, lg, AF.Exp, ["lg", "nm1"], ["ex"], bias=nm1, scale=1.0)
            tt(ex, ex, eq, ALU.mult, ["ex", "eq"], ["ex"])
            S.add("dve", lambda e: e.reduce_sum(out=den, in_=ex, axis=AX.X), r=["ex"], w=["den"])
            S.add("dve", lambda e: e.reciprocal(out=den, in_=den), r=["den"], w=["den"])
            ts(gates[:, t, :], ex, den, ALU.mult, ["ex", "den"], ["gates"])

        _fwi = [0]

        def ffn_group(wg_src, wu_src, wd_src, c0, G, gate_col, pre_tg=None):
            s_ = _fwi[0] % 2
            _fwi[0] += 1
            wg, wu, wd = FW[s_]
            dma("pool", wg[:, :, 0:G * 128], wg_src.rearrange("(k p) c -> p k c", p=128)[:, :, c0 * 128:(c0 + G) * 128], [], [f"fw{s_}0"], s_fw[s_][0])
            dma("pool", wu[:, :, 0:G * 128], wu_src.rearrange("(k p) c -> p k c", p=128)[:, :, c0 * 128:(c0 + G) * 128], [], [f"fw{s_}1"], s_fw[s_][1])
            dma("pool", wd[:, 0:G, :], wd_src[c0 * 128:(c0 + G) * 128, :].rearrange("(g p) c -> p g c", p=128), [], [f"fw{s_}2"], s_fw[s_][2])
            for tg in range(4):
                ab = tg % 2
                if pre_tg is not None:
                    pre_tg(tg)
                for j in range(G):
                    pg, pgn = nextF()
                    for k in range(8):
                        mm(pg[:], wg[:, k, j * 128:(j + 1) * 128], fT[:, k, tg * 512:(tg + 1) * 512], k == 0, k == 7, [f"fw{s_}0", f"fT{tg}"], [pgn])
                    pu, pun = nextF()
                    for k in range(8):
                        mm(pu[:], wu[:, k, j * 128:(j + 1) * 128], fT[:, k, tg * 512:(tg + 1) * 512], k == 0, k == 7, [f"fw{s_}1", f"fT{tg}"], [pun])
                    act(sg[:, j % 2, :], pg[:], AF.Silu, [pgn], [f"sg{j % 2}"])
                    tt(actT[:, ab, j, :], sg[:, j % 2, :], pu[:], ALU.mult, [f"sg{j % 2}", pun], [f"actT{ab}"])
                for tt_ in range(4):
                    t = tg * 4 + tt_
                    for half in range(2):
                        pd, pdn = nextF()
                        for j in range(G):
                            mm(pd[:], actT[:, ab, j, tt_ * 128:(tt_ + 1) * 128], wd[:, j, half * 512:(half + 1) * 512], j == 0, j == G - 1, [f"actT{ab}", f"fw{s_}2"], [pdn])
                        hs = h[:, t, half * 512:(half + 1) * 512]
                        if gate_col is None:
                            tt(hs, pd[:], hs, ALU.add, [pdn, f"h{t}"], [f"h{t}"])
                        else:
                            stt(hs, pd[:], gates[:, t, gate_col:gate_col + 1], hs, ALU.mult, ALU.add, [pdn, "gates", f"h{t}"], [f"h{t}"])

        def ffn(l):
            load_gvec(2 + l)
            moe = (l == 1)
            first = [True]

            def pre(tg):
                ffn_norm_tiles(moe, range(4 * tg, 4 * tg + 4))

            if not moe:
                for (c0, G) in FF_GROUPS:
                    ffn_group(dwg_d[0], dwu_d[0], dwd_d[0], c0, G, None, pre_tg=pre if first[0] else None)
                    first[0] = False
            else:
                for e_ in range(NE):
                    for (c0, G) in FF_GROUPS:
                        ffn_group(mwg_d[0, e_], mwu_d[0, e_], mwd_d[0, e_], c0, G, e_, pre_tg=pre if first[0] else None)
                        first[0] = False

        s_out = [S.dsem(f"out{i}") for i in range(2)]

        def final():
            load_gvec(4)
            for t in range(NT):
                i = t % 2
                ss = small[:, 0:1]
                lnv = small[:, 1:2]
                rstd = small[:, 2:3]
                act(a_bf[:], h[:, t, :], AF.Square, [f"h{t}"], ["a_bf", "ss"], accum=ss)
                act(lnv, ss, AF.Ln, ["ss"], ["lnv"], bias=EPS, scale=1.0 / D)
                act(rstd, lnv, AF.Exp, ["lnv"], ["rstd"], scale=-0.5)
                stt(outst[:, i, :], h[:, t, :], rstd, gvec[:], ALU.mult, ALU.mult, [f"h{t}", "rstd", "gvec"], [f"outst{i}"])
                dma("sp", y_d[t * 128:(t + 1) * 128, :], outst[:, i, :], [f"outst{i}"], [f"y{t}"], s_out[i])
            S.add("sp", None, r=[f"y{t}" for t in range(NT)], w=[])

        def dump_h():
            S.barrier()
            sd = S.dsem("dump")
            for t in range(NT):
                dma("sp", y_d[t * 128:(t + 1) * 128, :], h[:, t, :], [f"h{t}"], [f"y{t}"], sd)
            S.add("sp", None, r=[f"y{t}" for t in range(NT)], w=[])

        def program():
            load_x(xp_d if SI >= 1 else x_d)
            if SI == 0:
                return dump_h()
            mixer_main(0, None)
            save_state(0)
            if stop == 'mixA':
                return dump_h()
            S.barrier()
            ffn(0)
            if stop == 'ffnA':
                return dump_h()
            S.barrier()
            mixer_prepass(1)
            save_state(1)
            if stop == 'preA':
                return dump_h()
            S.barrier()
            load_x(x_d)
            for l in range(2):
                mixer_main(l, l)
                if stop == f'mix{l}':
                    return dump_h()
                S.barrier()
                ffn(l)
                if stop == f'ffn{l}':
                    return dump_h()
                S.barrier()
            final()

        program()
        print("ops:", {e: (len(S.ops[e]), sum(1 for o in S.ops[e] if o.mark)) for e in S.ENGS})
        S.emit(nc, stack)
    return nc


_CACHE = {}


def _prep_shared(inp):
    f = lambda a: np.ascontiguousarray(np.asarray(a, dtype=np.float32))
    consts = np.zeros((128, C_N), np.float32)
    consts[:, C_ID:C_ID + 128] = np.eye(128, dtype=np.float32)
    consts[:, C_ONES:C_ONES + 128] = 1.0 / 256.0
    p = np.arange(128)[:, None] % 64
    i = np.arange(64)[None, :]
    consts[:, C_TRI:C_TRI + 64] = (i >= p).astype(np.float32)
    sm = np.ones((128, 256), np.float32)
    sm[:, 0::64] = 0.0
    consts[:, C_SM:C_SM + 256] = sm
    pvec = np.zeros((2, 128, PV_N), np.float32)
    for l in range(2):
        scw = f(inp["sc_conv_w"][l])
        cfw = f(inp["cf_conv_w"][l])
        for cc in range(2):
            pvec[l, :, PV_SCW + cc * 3:PV_SCW + cc * 3 + 3] = scw[:, cc * 128:(cc + 1) * 128].T
            pvec[l, :, PV_CFW + cc * 31:PV_CFW + cc * 31 + 31] = cfw[:, cc * 128:(cc + 1) * 128].T
            pvec[l, :, PV_CFB + cc] = f(inp["cf_conv_b"][l])[cc * 128:(cc + 1) * 128]
            pvec[l, :, PV_LNG + cc] = f(inp["cf_ln_g"][l])[cc * 128:(cc + 1) * 128]
            pvec[l, :, PV_LNB + cc] = f(inp["cf_ln_b"][l])[cc * 128:(cc + 1) * 128]
            pvec[l, :, PV_BA + cc] = f(inp["gla_b_a"][l])[cc * 128:(cc + 1) * 128]
    bvec = np.zeros((5, 128, D), np.float32)
    bvec[0] = f(inp["attn_norm_g"][0])[None, :]
    bvec[1] = f(inp["attn_norm_g"][1])[None, :]
    bvec[2] = f(inp["ffn_norm_g"][0])[None, :]
    bvec[3] = f(inp["ffn_norm_g"][1])[None, :]
    bvec[4] = f(inp["final_norm_g"])[None, :]
    gng = np.zeros((2, 128, 512), np.float32)
    for l in range(2):
        gng[l] = f(inp["gla_norm_g"][l]).reshape(1, 512)
    shared = {
        "consts": consts, "pvec": pvec, "bvec": bvec, "gng": gng,
        "w_a2": f(inp["gla_w_a2"]), "w_in": f(inp["w_in"]), "w_out": f(inp["w_out"]),
        "dense_w_gate": f(inp["dense_w_gate"]), "dense_w_up": f(inp["dense_w_up"]), "dense_w_down": f(inp["dense_w_down"]),
        "moe_w_router": f(inp["moe_w_router"]), "moe_w_gate": f(inp["moe_w_gate"]), "moe_w_up": f(inp["moe_w_up"]),
        "moe_w_down": f(inp["moe_w_down"]),
    }
    return shared


def kernel(_stop=None, **inputs):
    if _stop is None:
        _stop = DBG_STOP
    x = np.asarray(inputs["x"], dtype=np.float32)
    shared = _prep_shared(inputs)
    if STAGES.index(_stop) < STAGES.index('ffn1'):
        for k_ in ("moe_w_gate", "moe_w_up", "moe_w_down"):
            shared.pop(k_)
    if _stop not in _CACHE:
        _CACHE[_stop] = build_program(_stop)
    nc = _CACHE[_stop]
    in_maps = []
    for r in range(NCORES):
        b, half = r // 2, r % 2
        sel = np.zeros((128, NCORES), np.float32)
        sel[:, 0] = float(half)
        m = dict(shared)
        m["x"] = np.ascontiguousarray(x[b, half * T:(half + 1) * T, :])
        m["x_prev"] = np.ascontiguousarray(x[b, 0:T, :]) if half == 1 else np.zeros((T, D), np.float32)
        m["sel"] = sel
        in_maps.append(m)
    res = run_bass_kernel_spmd(nc, in_maps, core_ids=list(range(NCORES)))
    out = np.zeros((4, 4096, D), np.float32)
    for r in range(NCORES):
        b, half = r // 2, r % 2
        out[b, half * T:(half + 1) * T, :] = np.asarray(res.results[r]["y"], dtype=np.float32)
    return out
```

```python
import contextlib
import os
DBG_EX = os.environ.get('DBG_EX', '')
DBG_MB = int(os.environ.get('DBG_MB', '99'))
DBG_G = os.environ.get('DBG_G', '')
DBG_H = os.environ.get('DBG_H', '')
DBG_Y = os.environ.get('DBG_Y', '')
DBG_STOP = os.environ.get('DBG_STOP', 'final')
import numpy as np
import concourse.bass as bass
import concourse.mybir as mybir
from concourse.bass_utils import run_bass_kernel_spmd

F32 = mybir.dt.float32
BF16 = mybir.dt.bfloat16
AF = mybir.ActivationFunctionType
ALU = mybir.AluOpType
AX = mybir.AxisListType

NCORES = 8
D = 1024
T = 2048
NT = 16
TB = 256
NBLK = T // TB
HALO = 32
EXT = HALO + TB
D_IN = 2832
D_FF = 2816
NE = 8
EPS = 1e-6
O_SCB, O_SCC, O_SCV, O_CFA, O_CFG, O_Q, O_K, O_V, O_ALR, O_R = 0, 256, 512, 768, 1024, 1280, 1536, 1792, 2304, 2320
FF_GROUPS = [(0, 4), (4, 4), (8, 4), (12, 4), (16, 4), (20, 2)]
PV_SCW, PV_CFW, PV_CFB, PV_LNG, PV_LNB, PV_BA, PV_N = 0, 6, 68, 70, 72, 74, 76
C_ID, C_ONES, C_TRI, C_SM, C_N = 0, 128, 256, 320, 576


class DSem:
    def __init__(self, name):
        self.name = name
        self.count = 0
        self.handle = None


class Op:
    __slots__ = ("eng", "fn", "waits", "mark", "semval", "idx", "dma")

    def __init__(self, eng, fn, dma):
        self.eng, self.fn, self.dma = eng, fn, dma
        self.waits = []
        self.mark = False
        self.semval = None
        self.idx = 0


class Sched:
    ENGS = ["pe", "act", "dve", "pool", "sp"]

    def __init__(self):
        self.ops = {e: [] for e in self.ENGS}
        self.res = {}
        self.pending = {e: [] for e in self.ENGS}
        self.dsems = []

    def dsem(self, name):
        s = DSem(name)
        self.dsems.append(s)
        return s

    def add(self, eng, fn, r=(), w=(), dma=None, rg=None):
        op = Op(eng, fn, dma)
        op.idx = len(self.ops[eng])
        deps = []
        force = None
        if eng == "pe":
            prev = getattr(self, "_pe_rg", None)
            if rg is not None and prev is not None and (rg[0] + rg[1] <= prev[0] or prev[0] + prev[1] <= rg[0]):
                force = self.ops["pe"][-1]
            self._pe_rg = rg
        for x in r:
            st = self.res.get(x)
            if st is not None and st[0] is not None:
                deps.append(st[0])
        for x in w:
            st = self.res.get(x)
            if st is not None:
                if st[0] is not None:
                    deps.append(st[0])
                deps.extend(st[1])
        deps.extend(self.pending[eng])
        self.pending[eng] = []
        if force is not None:
            force.mark = True
            op.waits.append(("op", force))
        seen = set()
        for tok in deps:
            if id(tok) in seen:
                continue
            seen.add(id(tok))
            if tok[0] == "op":
                p = tok[1]
                if p.eng == eng:
                    if eng == "pe":
                        continue
                    if dma is None and (op.idx - p.idx) > 2:
                        continue
                p.mark = True
            op.waits.append(tok)
        if dma is not None:
            dma.count += 1
            tok = ("dma", dma, dma.count * 16)
        else:
            tok = ("op", op)
        for x in r:
            st = self.res.setdefault(x, [None, []])
            st[1].append(tok)
        for x in w:
            self.res[x] = [tok, []]
        self.ops[eng].append(op)
        return op

    def barrier(self):
        toks = []
        for e in self.ENGS:
            if self.ops[e]:
                last = None
                for o in reversed(self.ops[e]):
                    if o.dma is None and o.fn is not None:
                        last = o
                        break
                if last is not None:
                    last.mark = True
                    toks.append(("op", last))
        for s in self.dsems:
            if s.count:
                toks.append(("dma", s, s.count * 16))
        for e in self.ENGS:
            self.pending[e] = [t for t in toks if not (t[0] == "op" and t[1].eng == e and e == "pe")]
        self.res = {}

    def emit(self, nc, stack):
        EPOCH = 30000
        engsems = {}
        for e in self.ENGS:
            n = 0
            for o in self.ops[e]:
                if o.mark:
                    n += 1
                    o.semval = n
            nep = max(1, (n + EPOCH - 1) // EPOCH)
            engsems[e] = [stack.enter_context(nc.semaphore(f"s_{e}{i}")) for i in range(nep)]
        for s in self.dsems:
            if s.count:
                s.handle = stack.enter_context(nc.semaphore("d_" + s.name))

        def semof(e, val):
            i = (val - 1) // EPOCH
            return engsems[e][i], val - i * EPOCH

        block = stack.enter_context(nc.Block())

        def run(ename):
            def body(eng):
                waited = {}
                for o in self.ops[ename]:
                    for tok in o.waits:
                        if tok[0] == "op":
                            sem, val = semof(tok[1].eng, tok[1].semval)
                        else:
                            sem, val = tok[1].handle, tok[2]
                        key = id(sem)
                        if waited.get(key, 0) >= val:
                            continue
                        waited[key] = val
                        eng.wait_ge(sem, val)
                    if o.fn is None:
                        continue
                    ins = o.fn(eng)
                    if o.dma is not None:
                        ins.then_inc(o.dma.handle, 16)
                    elif o.mark:
                        sem, _ = semof(ename, o.semval)
                        ins.then_inc(sem, 1)
            return body

        block.tensor(run("pe"))
        block.scalar(run("act"))
        block.vector(run("dve"))
        block.gpsimd(run("pool"))
        block.sync(run("sp"))


STAGES = ['load', 'mixA', 'ffnA', 'preA', 'mix0', 'ffn0', 'mix1', 'ffn1', 'final']


def build_program(stop='final'):
    nc = bass.Bass("TRN2", target_bir_lowering=False)
    S = Sched()
    SI = STAGES.index(stop)
    need_moe = SI >= STAGES.index('ffn1')

    def din(name, shape):
        return nc.dram_tensor(name, list(shape), F32, kind="ExternalInput").ap()

    x_d = din("x", [T, D])
    xp_d = din("x_prev", [T, D])
    sel_d = din("sel", [128, NCORES])
    consts_d = din("consts", [128, C_N])
    pvec_d = din("pvec", [2, 128, PV_N])
    bvec_d = din("bvec", [5, 128, D])
    gng_d = din("gng", [2, 128, 512])
    wa2_d = din("w_a2", [2, 16, 256])
    win_d = din("w_in", [2, D, D_IN])
    wout_d = din("w_out", [2, D, D])
    dwg_d = din("dense_w_gate", [1, D, D_FF])
    dwu_d = din("dense_w_up", [1, D, D_FF])
    dwd_d = din("dense_w_down", [1, D_FF, D])
    wr_d = din("moe_w_router", [1, D, NE])
    if need_moe:
        mwg_d = din("moe_w_gate", [1, NE, D, D_FF])
        mwu_d = din("moe_w_up", [1, NE, D, D_FF])
        mwd_d = din("moe_w_down", [1, NE, D_FF, D])
    y_d = nc.dram_tensor("y", [T, D], F32, kind="ExternalOutput").ap()

    stack = contextlib.ExitStack()
    with stack:
        def sb(name, shape, dt=F32):
            return stack.enter_context(nc.sbuf_tensor("sb_" + name, list(shape), dt))

        h = sb("h", [128, NT, D])
        wbuf = sb("wbuf", [128, 8 * D_IN + 8 * D], BF16)
        W_in = wbuf[:, 0:8 * D_IN].rearrange("p (k c) -> p k c", k=8)
        W_out = wbuf[:, 8 * D_IN:8 * D_IN + 8 * D].rearrange("p (k c) -> p k c", k=8)
        FW = []
        for s_ in range(2):
            base = s_ * 12288
            wg = wbuf[:, base:base + 4096].rearrange("p (k c) -> p k c", k=8)
            wu = wbuf[:, base + 4096:base + 8192].rearrange("p (k c) -> p k c", k=8)
            wd = wbuf[:, base + 8192:base + 12288].rearrange("p (g c) -> p g c", g=4)
            FW.append((wg, wu, wd))
        RR_N = 10240
        RR = sb("RR", [128, RR_N], F32)
        fT = RR[:, 0:8192].bitcast(BF16).rearrange("p (k t) -> p k t", k=8)
        _off = [0]

        def carve(n_f32, dt=F32, shape=None):
            a = RR[:, _off[0]:_off[0] + n_f32]
            _off[0] += n_f32
            if dt == BF16:
                a = a.bitcast(BF16)
            return a

        S_A = carve(256).rearrange("p (a t) -> p a t", a=2)
        S_T = carve(256).rearrange("p (a t) -> p a t", a=2)
        S_bf = carve(128, BF16).rearrange("p (a t) -> p a t", a=2)
        _x0 = _off[0]
        csp = carve(512).rearrange("p (a t) -> p a t", a=2)
        E_ = carve(512).rearrange("p (a t) -> p a t", a=2)
        Ei = carve(512).rearrange("p (a t) -> p a t", a=2)
        qtT = carve(256, BF16).rearrange("p (a t) -> p a t", a=2)
        ktT = carve(256, BF16).rearrange("p (a t) -> p a t", a=2)
        kt = carve(256, BF16).rearrange("p (a t) -> p a t", a=2)
        vv = carve(512, BF16).rearrange("p (a t) -> p a t", a=2)
        sr = carve(512, BF16).rearrange("p (a t) -> p a t", a=2)
        m_ = carve(2 * EXT).rearrange("p (a t) -> p a t", a=2)
        u_ = carve(EXT, BF16).rearrange("p (a t) -> p a t", a=2)
        tmpx = carve(EXT)
        acc_sc = carve(256)
        cfc = carve(512).rearrange("p (a t) -> p a t", a=2)
        cfsq = carve(512).rearrange("p (a t) -> p a t", a=2)
        mean_sb = carve(256)
        var_sb = carve(256)
        rstd_sb = carve(256)
        yT = carve(1024, BF16).rearrange("p (a t) -> p a t", a=8)
        o_sb = carve(512)
        og = carve(512)
        ogb = carve(256, BF16)
        AT = carve(128, BF16).rearrange("p (a t) -> p a t", a=4)
        alrT = carve(256)
        assert _off[0] <= RR_N, _off[0]
        _off[0] = _x0
        stS = carve(256)
        stH = carve(1024)
        haloh = carve(1024)
        outst = RR[:, 0:2048].rearrange("p (a t) -> p a t", a=2)

        aTx = sb("aTx", [128, 8, EXT], BF16)
        FR = sb("FR", [128, 4608], F32)
        actT = FR[:, 0:2048].bitcast(BF16).rearrange("p (a j t) -> p a j t", a=2, j=4)
        f32t = FR[:, 2048:3072]
        f32T = FR[:, 3072:4096].rearrange("p (k t) -> p k t", k=8)
        sg = FR[:, 4096:4608].bitcast(BF16).rearrange("p (a t) -> p a t", a=2)
        D_cf = FR[:, 0:3968].bitcast(BF16).rearrange("p (j c) -> p j c", j=62)
        aTx1 = FR[:, 0:1152].bitcast(BF16).rearrange("p (k t) -> p k t", k=8)
        csp1 = FR[:, 1152:1664].rearrange("p (a t) -> p a t", a=2)
        E_1 = FR[:, 1664:2176].rearrange("p (a t) -> p a t", a=2)
        Ei1 = FR[:, 2176:2688].rearrange("p (a t) -> p a t", a=2)
        a_bf = sb("a_bf", [128, D], BF16)
        gvec = sb("gvec", [128, D])
        consts = sb("consts", [128, C_N])
        ident_bf = sb("ident_bf", [128, 128], BF16)
        pvec = sb("pvec", [128, 2, PV_N])
        nba = sb("nba", [128, 2, 2])
        gng = sb("gng", [128, 2, 512])
        wa2 = sb("wa2", [16, 2, 256])
        wr = sb("wr", [128, 8, NE])
        sel = sb("sel", [128, NCORES])
        small = sb("small", [128, 64])
        Ssave = sb("Ssave", [128, 2, 256])
        Hsave = sb("Hsave", [128, 2, 8 * HALO], BF16)
        gates = sb("gates", [128, NT, NE])

        psF = [stack.enter_context(nc.psum_tensor(f"psF{i}", [128, 512], F32)) for i in range(6)]
        psB = [stack.enter_context(nc.psum_tensor(f"psB{i}", [128, 1024], BF16)) for i in range(2)]
        _rr = [0, 0]

        def nextF():
            i = _rr[0] % 5
            _rr[0] += 1
            return psF[i], f"F{i}"

        def nextB():
            i = _rr[1] % 2
            _rr[1] += 1
            return psB[i], f"B{i}"

        ident = consts[:, C_ID:C_ID + 128]
        ones256 = consts[:, C_ONES:C_ONES + 128]
        tri = consts[:, C_TRI:C_TRI + 64]
        smask = consts[:, C_SM:C_SM + 256]

        def mm(out, lhsT, rhs, start, stop, r, w):
            rg = (lhsT.base_partition(), lhsT.partition_size())
            S.add("pe", lambda e: e.matmul(out, lhsT=lhsT, rhs=rhs, start=start, stop=stop), r=r, w=w, rg=rg)

        def tr(out, in_, idn, r, w):
            rg = (in_.base_partition(), in_.partition_size())
            S.add("pe", lambda e: e.transpose(out=out, in_=in_, identity=idn), r=r, w=w, rg=rg)

        def act(out, in_, func, r, w, bias=None, scale=None, accum=None):
            kw = {}
            if bias is not None:
                kw["bias"] = bias
            if scale is not None:
                kw["scale"] = scale
            if accum is not None:
                kw["accum_out"] = accum
            S.add("act", lambda e: e.activation(out=out, in_=in_, func=func, **kw), r=r, w=w)

        def tt(out, in0, in1, op, r, w, eng="dve"):
            S.add(eng, lambda e: e.tensor_tensor(out=out, in0=in0, in1=in1, op=op), r=r, w=w)

        def ts(out, in0, s1, op0, r, w, s2=None, op1=None, eng="dve"):
            if op1 is None:
                S.add(eng, lambda e: e.tensor_scalar(out=out, in0=in0, scalar1=s1, scalar2=None, op0=op0), r=r, w=w)
            else:
                S.add(eng, lambda e: e.tensor_scalar(out=out, in0=in0, scalar1=s1, scalar2=s2, op0=op0, op1=op1), r=r, w=w)

        def stt(out, in0, scalar, in1, op0, op1, r, w):
            S.add("dve", lambda e: e.scalar_tensor_tensor(out=out, in0=in0, scalar=scalar, in1=in1, op0=op0, op1=op1), r=r, w=w)

        def cp(out, in_, r, w, eng="dve"):
            S.add(eng, lambda e: e.tensor_copy(out=out, in_=in_), r=r, w=w)

        def dma(q, out, in_, r, w, sem):
            S.add(q, lambda e: e.dma_start(out=out, in_=in_), r=r, w=w, dma=sem)

        s_misc = [S.dsem(f"misc{i}") for i in range(10)]
        dma("sp", consts[:], consts_d, [], ["consts"], s_misc[0])
        dma("pool", ident_bf[:], consts_d[:, C_ID:C_ID + 128], [], ["ident_bf"], s_misc[1])
        dma("sp", pvec[:], pvec_d.rearrange("l p n -> p l n"), [], ["pvec"], s_misc[2])
        dma("sp", gng[:], gng_d.rearrange("l p n -> p l n"), [], ["gng"], s_misc[3])
        dma("sp", wa2[:], wa2_d.rearrange("l r n -> r l n"), [], ["wa2"], s_misc[4])
        dma("sp", wr[:], wr_d[0].rearrange("(k p) e -> p k e", p=128), [], ["wr"], s_misc[5])
        dma("sp", sel[:], sel_d, [], ["sel"], s_misc[6])
        s_x = [S.dsem(f"x{t}") for t in range(NT)]

        def load_x(src):
            for t in range(NT):
                dma("sp", h[:, t, :], src[t * 128:(t + 1) * 128, :], [], [f"h{t}"], s_x[t])
        ts(nba[:], pvec[:, :, PV_BA:PV_BA + 2], -1.0, ALU.mult, ["pvec"], ["nba"])

        s_win = [S.dsem(f"win{k}") for k in range(8)]
        s_wout = S.dsem("wout")
        s_gv = S.dsem("gv")
        s_fw = [[S.dsem(f"fw{s_}{j}") for j in range(3)] for s_ in range(2)]

        def load_mixer_weights(l):
            for k in range(8):
                dma("pool", W_in[:, k, :], win_d[l, k * 128:(k + 1) * 128, :], [], [f"Win{k}"] + [f"fw{s_}{j}" for s_ in range(2) for j in range(3)], s_win[k])
            dma("pool", W_out[:], wout_d[l].rearrange("(k p) c -> p k c", p=128), [], ["Wout"], s_wout)

        def norm_tile(src, src_res, nrows, gidx_res, dstT, dst_res, want_f32=False):
            ss = small[0:nrows, 0:1]
            lnv = small[0:nrows, 1:2]
            rstd = small[0:nrows, 2:3]
            act(a_bf[0:nrows, :], src, AF.Square, [src_res], ["a_bf", "ss"], accum=ss)
            act(lnv, ss, AF.Ln, ["ss"], ["lnv"], bias=EPS, scale=1.0 / D)
            act(rstd, lnv, AF.Exp, ["lnv"], ["rstd"], scale=-0.5)
            if want_f32:
                stt(f32t[0:nrows, :], src, rstd, gvec[0:nrows, :], ALU.mult, ALU.mult, [src_res, "rstd", gidx_res], ["f32t"])
                act(a_bf[0:nrows, :], f32t[0:nrows, :], AF.Copy, ["f32t"], ["a_bf"])
            else:
                stt(a_bf[0:nrows, :], src, rstd, gvec[0:nrows, :], ALU.mult, ALU.mult, [src_res, "rstd", gidx_res], ["a_bf"])
            pb, pbn = nextB()
            for k in range(8):
                tr(pb[:, k * nrows:(k + 1) * nrows], a_bf[0:nrows, k * 128:(k + 1) * 128], ident_bf[0:nrows, 0:nrows],
                   ["a_bf", "ident_bf"], [pbn])
            cp(dstT, pb[:, 0:8 * nrows].rearrange("p (k t) -> p k t", k=8), [pbn], [dst_res], eng="act" if False else "dve")

        def load_gvec(i):
            dma("sp", gvec[:], bvec_d[i], [], ["gvec"], s_gv)

        class BS:
            pass

        PB = []
        for i_, (a_, c_, e_, ei_) in enumerate([(aTx, csp, E_, Ei), (aTx, csp, E_, Ei)]):
            o_ = BS()
            o_.aTx, o_.csp, o_.E, o_.Ei = a_, c_, e_, ei_
            o_.n_aTx, o_.n_csp, o_.n_E, o_.n_Ei = "aTx0", "csp0", "E0", "Ei0"
            PB.append(o_)

        def proj_fm(col0, ncols, rhs, rhs_res, N):
            p, pn = nextF()
            for k in range(8):
                mm(p[0:ncols, 0:N], W_in[:, k, col0:col0 + ncols], rhs[:, k, :], k == 0, k == 7, [f"Win{k}", rhs_res], [pn])
            return p, pn

        def prep_stream(l, b, P, Pprev):
            if Pprev is not None:
                cp(P.aTx[:, :, 0:HALO], Pprev.aTx[:, :, EXT - HALO:EXT], [Pprev.n_aTx], [P.n_aTx])
            for ti in range(2):
                t = b * 2 + ti
                norm_tile(h[:, t, :], f"h{t}", 128, "gvec", P.aTx[:, :, HALO + ti * 128:HALO + (ti + 1) * 128], P.n_aTx)
                yield
            rhs = P.aTx[:, :, HALO:EXT]
            p, pn = proj_fm(O_ALR, 16, rhs, P.n_aTx, TB)
            cp(alrT[0:16, :], p[0:16, 0:TB], [pn], ["alrT"])
            yield
            pz, pzn = nextF()
            for hp in range(2):
                mm(pz[:, hp * 256:(hp + 1) * 256], wa2[0:16, l, hp * 128:(hp + 1) * 128], alrT[0:16, :], True, True, ["wa2", "alrT"], [pzn])
            for hp in range(2):
                act(P.csp[:, hp, :], pz[:, hp * 256:(hp + 1) * 256], AF.Exp, [pzn, "nba"], [P.n_csp], bias=nba[:, l, hp:hp + 1], scale=-1.0)
            yield
            act(P.csp[:], P.csp[:], AF.Ln, [P.n_csp], [P.n_csp], bias=1.0)
            for hp in range(2):
                S.add("dve", (lambda hp_: (lambda e: e.tensor_tensor_scan(out=P.csp[:, hp_, :], data0=smask, data1=P.csp[:, hp_, :], initial=0.0, op0=ALU.mult, op1=ALU.add)))(hp),
                      r=[P.n_csp, "consts"], w=[P.n_csp])
            yield
            act(P.Ei[:], P.csp[:], AF.Exp, [P.n_csp], [P.n_Ei], scale=1.0 / 16.0)
            act(P.E[:], P.csp[:], AF.Exp, [P.n_csp], [P.n_E], scale=-1.0 / 16.0)
            yield

        def kv_block(P):
            rhs = P.aTx[:, :, HALO:EXT]
            pk, pkn = nextF()
            for hp in range(2):
                for k in range(8):
                    mm(pk[:, hp * 256:(hp + 1) * 256], W_in[:, k, O_K + hp * 128:O_K + (hp + 1) * 128], rhs[:, k, :], k == 0, k == 7, [f"Win{k}", P.n_aTx], [pkn])
            tt(ktT[:], pk[:].rearrange("p (a t) -> p a t", a=2), P.Ei[:], ALU.mult, [pkn, P.n_Ei], ["ktT"])
            pb, pbn = nextB()
            for ti in range(2):
                for hp in range(2):
                    tr(pb[:, ti * 256 + hp * 128: ti * 256 + (hp + 1) * 128], ktT[:, hp, ti * 128:(ti + 1) * 128], ident_bf[:], ["ktT", "ident_bf"], [pbn])
            cp(kt[:], pb[:, 0:512].rearrange("p (a t) -> p a t", a=2), [pbn], ["kt"])
            for ti in range(2):
                pv, pvn = nextF()
                for k in range(8):
                    mm(pv[:], P.aTx[:, k, HALO + ti * 128:HALO + (ti + 1) * 128], W_in[:, k, O_V:O_V + 512], k == 0, k == 7, [f"Win{k}", P.n_aTx], [pvn])
                act(vv[:, ti, :], pv[:], AF.Copy, [pvn], ["vv"])

        def state_update(c, P):
            ti, par = c // 2, c % 2
            rows = slice(par * 64, par * 64 + 64)
            pS, pSn = nextF()
            for hh in range(4):
                hp, hl = hh // 2, hh % 2
                mm(pS[hl * 64:(hl + 1) * 64, hp * 128:(hp + 1) * 128], kt[rows, ti, hh * 64:(hh + 1) * 64], vv[rows, ti, hh * 128:(hh + 1) * 128],
                   True, True, ["kt", "vv"], [pSn])
            tt(S_T[:], pS[:, 0:256].rearrange("p (a t) -> p a t", a=2), S_A[:], ALU.add, [pSn, "S_A"], ["S_T"])
            for hp in range(2):
                ts(S_A[:, hp, :], S_T[:, hp, :], P.E[:, hp, c * 64 + 63:c * 64 + 64], ALU.mult, ["S_T", P.n_E], ["S_A"])
            cp(S_bf[:], S_A[:], ["S_A"], ["S_bf"])

        def prepass_stream(P):
            kv_block(P)
            yield
            for c in range(4):
                state_update(c, P)
                yield

        def interleave(*gens):
            gens = [g_ for g_ in gens if g_ is not None]
            while gens:
                for g_ in list(gens):
                    try:
                        next(g_)
                    except StopIteration:
                        gens.remove(g_)

        def conv_stream(l, b, P):
            aX, nX = P.aTx, P.n_aTx
            rhs = aX[:, :, HALO:EXT]
            for cc in range(2):
                pc, pcn = nextF()
                for k in range(8):
                    mm(pc[:, 0:EXT], W_in[:, k, O_SCC + cc * 128:O_SCC + (cc + 1) * 128], aX[:, k, :], k == 0, k == 7, [f"Win{k}", nX], [pcn])
                pv, pvn = nextF()
                for k in range(8):
                    mm(pv[:, 0:EXT], W_in[:, k, O_SCV + cc * 128:O_SCV + (cc + 1) * 128], aX[:, k, :], k == 0, k == 7, [f"Win{k}", nX], [pvn])
                act(tmpx[:], pc[:, 0:EXT], AF.Copy, [pcn], ["tmpx"])
                tt(m_[:, cc, :], tmpx[:], pv[:, 0:EXT], ALU.mult, ["tmpx", pvn], ["m"])
                yield
                w0 = pvec[:, l, PV_SCW + cc * 3 + 0:PV_SCW + cc * 3 + 1]
                w1 = pvec[:, l, PV_SCW + cc * 3 + 1:PV_SCW + cc * 3 + 2]
                w2 = pvec[:, l, PV_SCW + cc * 3 + 2:PV_SCW + cc * 3 + 3]
                ts(acc_sc[:], m_[:, cc, HALO - 2:HALO - 2 + TB], w0, ALU.mult, ["m", "pvec"], ["acc_sc"])
                stt(acc_sc[:], m_[:, cc, HALO - 1:HALO - 1 + TB], w1, acc_sc[:], ALU.mult, ALU.add, ["m", "pvec", "acc_sc"], ["acc_sc"])
                stt(acc_sc[:], m_[:, cc, HALO:HALO + TB], w2, acc_sc[:], ALU.mult, ALU.add, ["m", "pvec", "acc_sc"], ["acc_sc"])
                pb_, pbn_ = proj_fm(O_SCB + cc * 128, 128, rhs, nX, TB)
                tt(yT[:, cc, :], acc_sc[:], pb_[:, 0:TB], ALU.mult, ["acc_sc", pbn_], ["yT_sc"])
                yield
            for cc in range(2):
                pa, pan = nextF()
                for k in range(8):
                    mm(pa[:, 0:EXT], W_in[:, k, O_CFA + cc * 128:O_CFA + (cc + 1) * 128], aX[:, k, :], k == 0, k == 7, [f"Win{k}", nX], [pan])
                pg, pgn = nextF()
                for k in range(8):
                    mm(pg[:, 0:EXT], W_in[:, k, O_CFG + cc * 128:O_CFG + (cc + 1) * 128], aX[:, k, :], k == 0, k == 7, [f"Win{k}", nX], [pgn])
                act(tmpx[:], pg[:, 0:EXT], AF.Tanh, [pgn], ["tmpx"], scale=0.5)
                stt(u_[:, cc, :], tmpx[:], 1.0, pa[:, 0:EXT], ALU.add, ALU.mult, ["tmpx", pan], ["u"])
                yield
                pcv, pcvn = nextF()
                for kk in range(31):
                    mm(pcv[:, 0:TB], D_cf[:, cc * 31 + kk, :], u_[:, cc, HALO - 30 + kk:HALO - 30 + kk + TB], kk == 0, kk == 30, ["Dcf", "u"], [pcvn])
                act(cfc[:, cc, :], pcv[:, 0:TB], AF.Identity, [pcvn, "pvec"], ["cfc"], bias=pvec[:, l, PV_CFB + cc:PV_CFB + cc + 1], scale=0.5)
                act(cfsq[:, cc, :], cfc[:, cc, :], AF.Square, ["cfc"], ["cfsq"])
                yield
            pm, pmn = nextF()
            for cc in range(2):
                mm(pm[:, 0:TB], ones256, cfc[:, cc, :], cc == 0, cc == 1, ["consts", "cfc"], [pmn])
            for cc in range(2):
                mm(pm[:, TB:2 * TB], ones256, cfsq[:, cc, :], cc == 0, cc == 1, ["consts", "cfsq"], [pmn])
            cp(mean_sb[:], pm[:, 0:TB], [pmn], ["mean"])
            tt(var_sb[:], mean_sb[:], mean_sb[:], ALU.mult, ["mean"], ["var"])
            tt(var_sb[:], pm[:, TB:2 * TB], var_sb[:], ALU.subtract, [pmn, "var"], ["var"])
            yield
            act(rstd_sb[:], var_sb[:], AF.Ln, ["var"], ["rstdc"], bias=EPS)
            act(rstd_sb[:], rstd_sb[:], AF.Exp, ["rstdc"], ["rstdc"], scale=-0.5)
            for cc in range(2):
                tt(cfc[:, cc, :], cfc[:, cc, :], mean_sb[:], ALU.subtract, ["cfc", "mean"], ["cfc"])
                tt(cfc[:, cc, :], cfc[:, cc, :], rstd_sb[:], ALU.mult, ["cfc", "rstdc"], ["cfc"])
                act(yT[:, 2 + cc, :], cfc[:, cc, :], AF.Silu, ["cfc", "pvec"], ["yT_cf"],
                    bias=pvec[:, l, PV_LNB + cc:PV_LNB + cc + 1], scale=pvec[:, l, PV_LNG + cc:PV_LNG + cc + 1])
                yield

        def gla_stream(l, b, P):
            aX, nX = P.aTx, P.n_aTx
            rhs = aX[:, :, HALO:EXT]
            pq, pqn = nextF()
            for hp in range(2):
                for k in range(8):
                    mm(pq[:, hp * 256:(hp + 1) * 256], W_in[:, k, O_Q + hp * 128:O_Q + (hp + 1) * 128], rhs[:, k, :], k == 0, k == 7, [f"Win{k}", nX], [pqn])
            stt(qtT[:], pq[:].rearrange("p (a t) -> p a t", a=2), 0.125, P.E[:], ALU.mult, ALU.mult, [pqn, P.n_E], ["qtT"])
            yield
            kv_block(P)
            yield
            for ti in range(2):
                pr, prn = nextF()
                for k in range(8):
                    mm(pr[:], aX[:, k, HALO + ti * 128:HALO + (ti + 1) * 128], W_in[:, k, O_R:O_R + 512], k == 0, k == 7, [f"Win{k}", nX], [prn])
                act(sr[:, ti, :], pr[:], AF.Silu, [prn], ["sr"])
                tt(sr[:, ti, :], sr[:, ti, :], gng[:, l, :], ALU.mult, ["sr", "gng"], ["sr"])
                yield
            po, pon = psF[5], "F5"
            for ti in range(2):
                for par in range(2):
                    c = ti * 2 + par
                    rows = slice(par * 64, par * 64 + 64)
                    cols = slice(c * 64, c * 64 + 64)
                    pA, pAn = nextF()
                    for hh in (0, 2, 1, 3):
                        hp, hl = hh // 2, hh % 2
                        fr = slice(hl * 64, hl * 64 + 64)
                        mm(pA[rows, hh * 64:(hh + 1) * 64], ktT[fr, hp, cols], qtT[fr, hp, cols], True, True, ["ktT", "qtT"], [pAn])
                    tt(AT[rows, :, :], pA[rows, 0:256].rearrange("p (a t) -> p a t", a=4), tri[rows, :].unsqueeze(1).to_broadcast([64, 4, 64]),
                       ALU.mult, [pAn, "consts"], ["AT"])
                    yield
                    for hh in ((0, 2, 1, 3) if par == 0 else (1, 3, 0, 2)):
                        hp, hl = hh // 2, hh % 2
                        fr = slice(hl * 64, hl * 64 + 64)
                        mm(po[rows, hh * 128:(hh + 1) * 128], qtT[fr, hp, cols], S_bf[fr, hp, :], True, False, ["qtT", "S_bf"], [pon])
                        mm(po[rows, hh * 128:(hh + 1) * 128], AT[rows, hh, :], vv[rows, ti, hh * 128:(hh + 1) * 128], False, True, ["AT", "vv"], [pon])
                    yield
                    state_update(c, P)
                    yield
                for hh in range(4):
                    act(og[:, hh * 128:(hh + 1) * 128], po[:, hh * 128:(hh + 1) * 128], AF.Square, [pon], ["og", "ss4"], accum=small[:, 8 + hh:9 + hh])
                act(small[:, 12:16], small[:, 8:12], AF.Ln, ["ss4"], ["ln4"], bias=EPS, scale=1.0 / 128.0)
                act(small[:, 16:20], small[:, 12:16], AF.Exp, ["ln4"], ["rstd4"], scale=-0.5)
                yield
                for hh in range(4):
                    stt(ogb[:, hh * 128:(hh + 1) * 128], po[:, hh * 128:(hh + 1) * 128], small[:, 16 + hh:17 + hh], sr[:, ti, hh * 128:(hh + 1) * 128],
                        ALU.mult, ALU.mult, [pon, "rstd4", "sr"], ["ogb"])
                pb, pbn = nextB()
                for j in range(4):
                    tr(pb[:, j * 128:(j + 1) * 128], ogb[:, j * 128:(j + 1) * 128], ident_bf[:], ["ogb", "ident_bf"], [pbn])
                cp(yT[:, 4:8, ti * 128:(ti + 1) * 128], pb[:, 0:512].rearrange("p (a t) -> p a t", a=4), [pbn], ["yT_gla"])
                yield

        def wout_block(b):
            for ti in range(2):
                t = b * 2 + ti
                for half in range(2):
                    pw, pwn = nextF()
                    for k in range(8):
                        mm(pw[:], yT[:, k, ti * 128:(ti + 1) * 128], W_out[:, k, half * 512:(half + 1) * 512], k == 0, k == 7,
                           ["yT_sc", "yT_cf", "yT_gla", "Wout"], [pwn])
                    tt(h[:, t, half * 512:(half + 1) * 512], pw[:], h[:, t, half * 512:(half + 1) * 512], ALU.add, [pwn, f"h{t}"], [f"h{t}"])

        def save_state(i):
            P = PB[(NBLK - 1) % 2]
            ts(Ssave[:, i, :], S_A[:].rearrange("p a t -> p (a t)"), sel[:, 0:1], ALU.mult, ["S_A", "sel"], [f"Ssave{i}"])
            ts(Hsave[:, i, :].rearrange("p (k t) -> p k t", k=8), P.aTx[:, :, EXT - HALO:EXT], sel[:, 0:1], ALU.mult, [P.n_aTx, "sel"], [f"Hsave{i}"])

        def mixer_main(l, src):
            load_gvec(l)
            load_mixer_weights(l)
            if src is None:
                S.add("dve", lambda e: e.memset(S_A[:], 0.0), r=[], w=["S_A"])
                S.add("dve", lambda e: e.memset(PB[0].aTx[:, :, 0:HALO], 0.0), r=[], w=[PB[0].n_aTx])
            else:
                cp(S_A[:].rearrange("p a t -> p (a t)"), Ssave[:, src, :], [f"Ssave{src}"], ["S_A"])
                cp(PB[0].aTx[:, :, 0:HALO], Hsave[:, src, :].rearrange("p (k t) -> p k t", k=8), [f"Hsave{src}"], [PB[0].n_aTx])
            cp(S_bf[:], S_A[:], ["S_A"], ["S_bf"])
            for j in range(62):
                ts(D_cf[:, j, :], ident_bf[:], pvec[:, l, PV_CFW + j:PV_CFW + j + 1], ALU.mult, ["ident_bf", "pvec"], ["Dcf"])
            P = PB[0]
            for b in range(NBLK):
                interleave(prep_stream(l, b, P, P if b > 0 else None))
                interleave(gla_stream(l, b, P), conv_stream(l, b, P))
                wout_block(b)

        def mixer_prepass(l):
            load_gvec(l)
            load_mixer_weights(l)
            S.add("dve", lambda e: e.memset(S_A[:], 0.0), r=[], w=["S_A"])
            P = PB[0]
            for b in range(NBLK):
                interleave(prep_stream(l, b, P, P if b > 0 else None))
                interleave(prepass_stream(P))

        def ffn_norm_tiles(moe, tiles):
            for t in tiles:
                norm_tile(h[:, t, :], f"h{t}", 128, "gvec", fT[:, :, t * 128:(t + 1) * 128], f"fT{t // 4}", want_f32=moe)
                if moe:
                    router_tile(t)

        def router_tile(t):
            pl, pln = nextF()
            for half in range(2):
                pt, ptn = nextF()
                for j in range(4):
                    k = half * 4 + j
                    tr(pt[:, j * 128:(j + 1) * 128], f32t[:, k * 128:(k + 1) * 128], ident, ["f32t", "consts"], [ptn])
                cp(f32T[:, half * 4:(half + 1) * 4, :], pt[:].rearrange("p (a t) -> p a t", a=4), [ptn], ["f32T"])
            for k in range(8):
                mm(pl[:, 0:NE], f32T[:, k, :], wr[:, k, :], k == 0, k == 7, ["f32T", "wr"], [pln])
            lg = small[:, 24:32]
            m1 = small[:, 32:33]
            m2 = small[:, 33:34]
            eq = small[:, 34:42]
            l2 = small[:, 42:50]
            ex = small[:, 50:58]
            den = small[:, 58:59]
            nm1 = small[:, 59:60]
            cp(lg, pl[:, 0:NE], [pln], ["lg"])
            S.add("dve", lambda e: e.reduce_max(out=m1, in_=lg, axis=AX.X), r=["lg"], w=["m1"])
            ts(eq, lg, m1, ALU.is_equal, ["lg", "m1"], ["eq"])
            stt(l2, eq, -1e30, lg, ALU.mult, ALU.add, ["eq", "lg"], ["l2"])
            S.add("dve", lambda e: e.reduce_max(out=m2, in_=l2, axis=AX.X), r=["l2"], w=["m2"])
            ts(eq, lg, m2, ALU.is_ge, ["lg", "m2"], ["eq"])
            ts(nm1, m1, -1.0, ALU.mult, ["m1"], ["nm1"])
            act(ex, lg, AF.Exp, ["lg", "nm1"], ["ex"], bias=nm1, scale=1.0)
            tt(ex, ex, eq, ALU.mult, ["ex", "eq"], ["ex"])
            S.add("dve", lambda e: e.reduce_sum(out=den, in_=ex, axis=AX.X), r=["ex"], w=["den"])
            S.add("dve", lambda e: e.reciprocal(out=den, in_=den), r=["den"], w=["den"])
            ts(gates[:, t, :], ex, den, ALU.mult, ["ex", "den"], ["gates"])

        _fwi = [0]

        def ffn_group(wg_src, wu_src, wd_src, c0, G, gate_col, pre_tg=None):
            s_ = _fwi[0] % 2
            _fwi[0] += 1
            wg, wu, wd = FW[s_]
            dma("pool", wg[:, :, 0:G * 128], wg_src.rearrange("(k p) c -> p k c", p=128)[:, :, c0 * 128:(c0 + G) * 128], [], [f"fw{s_}0"], s_fw[s_][0])
            dma("pool", wu[:, :, 0:G * 128], wu_src.rearrange("(k p) c -> p k c", p=128)[:, :, c0 * 128:(c0 + G) * 128], [], [f"fw{s_}1"], s_fw[s_][1])
            dma("pool", wd[:, 0:G, :], wd_src[c0 * 128:(c0 + G) * 128, :].rearrange("(g p) c -> p g c", p=128), [], [f"fw{s_}2"], s_fw[s_][2])
            for tg in range(4):
                ab = tg % 2
                if pre_tg is not None:
                    pre_tg(tg)
                for j in range(G):
                    pg, pgn = nextF()
                    for k in range(8):
                        mm(pg[:], wg[:, k, j * 128:(j + 1) * 128], fT[:, k, tg * 512:(tg + 1) * 512], k == 0, k == 7, [f"fw{s_}0", f"fT{tg}"], [pgn])
                    pu, pun = nextF()
                    for k in range(8):
                        mm(pu[:], wu[:, k, j * 128:(j + 1) * 128], fT[:, k, tg * 512:(tg + 1) * 512], k == 0, k == 7, [f"fw{s_}1", f"fT{tg}"], [pun])
                    act(sg[:, j % 2, :], pg[:], AF.Silu, [pgn], [f"sg{j % 2}"])
                    tt(actT[:, ab, j, :], sg[:, j % 2, :], pu[:], ALU.mult, [f"sg{j % 2}", pun], [f"actT{ab}"])
                for tt_ in range(4):
                    t = tg * 4 + tt_
                    for half in range(2):
                        pd, pdn = nextF()
                        for j in range(G):
                            mm(pd[:], actT[:, ab, j, tt_ * 128:(tt_ + 1) * 128], wd[:, j, half * 512:(half + 1) * 512], j == 0, j == G - 1, [f"actT{ab}", f"fw{s_}2"], [pdn])
                        hs = h[:, t, half * 512:(half + 1) * 512]
                        if gate_col is None:
                            tt(hs, pd[:], hs, ALU.add, [pdn, f"h{t}"], [f"h{t}"])
                        else:
                            stt(hs, pd[:], gates[:, t, gate_col:gate_col + 1], hs, ALU.mult, ALU.add, [pdn, "gates", f"h{t}"], [f"h{t}"])

        def ffn(l):
            load_gvec(2 + l)
            moe = (l == 1)
            first = [True]

            def pre(tg):
                ffn_norm_tiles(moe, range(4 * tg, 4 * tg + 4))

            if not moe:
                for (c0, G) in FF_GROUPS:
                    ffn_group(dwg_d[0], dwu_d[0], dwd_d[0], c0, G, None, pre_tg=pre if first[0] else None)
                    first[0] = False
            else:
                for e_ in range(NE):
                    for (c0, G) in FF_GROUPS:
                        ffn_group(mwg_d[0, e_], mwu_d[0, e_], mwd_d[0, e_], c0, G, e_, pre_tg=pre if first[0] else None)
                        first[0] = False

        s_out = [S.dsem(f"out{i}") for i in range(2)]

        def final():
            load_gvec(4)
            for t in range(NT):
                i = t % 2
                ss = small[:, 0:1]
                lnv = small[:, 1:2]
                rstd = small[:, 2:3]
                act(a_bf[:], h[:, t, :], AF.Square, [f"h{t}"], ["a_bf", "ss"], accum=ss)
                act(lnv, ss, AF.Ln, ["ss"], ["lnv"], bias=EPS, scale=1.0 / D)
                act(rstd, lnv, AF.Exp, ["lnv"], ["rstd"], scale=-0.5)
                stt(outst[:, i, :], h[:, t, :], rstd, gvec[:], ALU.mult, ALU.mult, [f"h{t}", "rstd", "gvec"], [f"outst{i}"])
                dma("sp", y_d[t * 128:(t + 1) * 128, :], outst[:, i, :], [f"outst{i}"], [f"y{t}"], s_out[i])
            S.add("sp", None, r=[f"y{t}" for t in range(NT)], w=[])

        def dump_h():
            S.barrier()
            sd = S.dsem("dump")
            for t in range(NT):
                dma("sp", y_d[t * 128:(t + 1) * 128, :], h[:, t, :], [f"h{t}"], [f"y{t}"], sd)
            S.add("sp", None, r=[f"y{t}" for t in range(NT)], w=[])

        def program():
            load_x(xp_d if SI >= 1 else x_d)
            if SI == 0:
                return dump_h()
            mixer_main(0, None)
            save_state(0)
            if stop == 'mixA':
                return dump_h()
            S.barrier()
            ffn(0)
            if stop == 'ffnA':
                return dump_h()
            S.barrier()
            mixer_prepass(1)
            save_state(1)
            if stop == 'preA':
                return dump_h()
            S.barrier()
            load_x(x_d)
            for l in range(2):
                mixer_main(l, l)
                if stop == f'mix{l}':
                    return dump_h()
                S.barrier()
                ffn(l)
                if stop == f'ffn{l}':
                    return dump_h()
                S.barrier()
            final()

        program()
        print("ops:", {e: (len(S.ops[e]), sum(1 for o in S.ops[e] if o.mark)) for e in S.ENGS})
        S.emit(nc, stack)
    return nc


_CACHE = {}


def _prep_shared(inp):
    f = lambda a: np.ascontiguousarray(np.asarray(a, dtype=np.float32))
    consts = np.zeros((128, C_N), np.float32)
    consts[:, C_ID:C_ID + 128] = np.eye(128, dtype=np.float32)
    consts[:, C_ONES:C_ONES + 128] = 1.0 / 256.0
    p = np.arange(128)[:, None] % 64
    i = np.arange(64)[None, :]
    consts[:, C_TRI:C_TRI + 64] = (i >= p).astype(np.float32)
    sm = np.ones((128, 256), np.float32)
    sm[:, 0::64] = 0.0
    consts[:, C_SM:C_SM + 256] = sm
    pvec = np.zeros((2, 128, PV_N), np.float32)
    for l in range(2):
        scw = f(inp["sc_conv_w"][l])
        cfw = f(inp["cf_conv_w"][l])
        for cc in range(2):
            pvec[l, :, PV_SCW + cc * 3:PV_SCW + cc * 3 + 3] = scw[:, cc * 128:(cc + 1) * 128].T
            pvec[l, :, PV_CFW + cc * 31:PV_CFW + cc * 31 + 31] = cfw[:, cc * 128:(cc + 1) * 128].T
            pvec[l, :, PV_CFB + cc] = f(inp["cf_conv_b"][l])[cc * 128:(cc + 1) * 128]
            pvec[l, :, PV_LNG + cc] = f(inp["cf_ln_g"][l])[cc * 128:(cc + 1) * 128]
            pvec[l, :, PV_LNB + cc] = f(inp["cf_ln_b"][l])[cc * 128:(cc + 1) * 128]
            pvec[l, :, PV_BA + cc] = f(inp["gla_b_a"][l])[cc * 128:(cc + 1) * 128]
    bvec = np.zeros((5, 128, D), np.float32)
    bvec[0] = f(inp["attn_norm_g"][0])[None, :]
    bvec[1] = f(inp["attn_norm_g"][1])[None, :]
    bvec[2] = f(inp["ffn_norm_g"][0])[None, :]
    bvec[3] = f(inp["ffn_norm_g"][1])[None, :]
    bvec[4] = f(inp["final_norm_g"])[None, :]
    gng = np.zeros((2, 128, 512), np.float32)
    for l in range(2):
        gng[l] = f(inp["gla_norm_g"][l]).reshape(1, 512)
    shared = {
        "consts": consts, "pvec": pvec, "bvec": bvec, "gng": gng,
        "w_a2": f(inp["gla_w_a2"]), "w_in": f(inp["w_in"]), "w_out": f(inp["w_out"]),
        "dense_w_gate": f(inp["dense_w_gate"]), "dense_w_up": f(inp["dense_w_up"]), "dense_w_down": f(inp["dense_w_down"]),
        "moe_w_router": f(inp["moe_w_router"]), "moe_w_gate": f(inp["moe_w_gate"]), "moe_w_up": f(inp["moe_w_up"]),
        "moe_w_down": f(inp["moe_w_down"]),
    }
    return shared


def kernel(_stop=None, **inputs):
    if _stop is None:
        _stop = DBG_STOP
    x = np.asarray(inputs["x"], dtype=np.float32)
    shared = _prep_shared(inputs)
    if STAGES.index(_stop) < STAGES.index('ffn1'):
        for k_ in ("moe_w_gate", "moe_w_up", "moe_w_down"):
            shared.pop(k_)
    if _stop not in _CACHE:
        _CACHE[_stop] = build_program(_stop)
    nc = _CACHE[_stop]
    in_maps = []
    for r in range(NCORES):
        b, half = r // 2, r % 2
        sel = np.zeros((128, NCORES), np.float32)
        sel[:, 0] = float(half)
        m = dict(shared)
        m["x"] = np.ascontiguousarray(x[b, half * T:(half + 1) * T, :])
        m["x_prev"] = np.ascontiguousarray(x[b, 0:T, :]) if half == 1 else np.zeros((T, D), np.float32)
        m["sel"] = sel
        in_maps.append(m)
    res = run_bass_kernel_spmd(nc, in_maps, core_ids=list(range(NCORES)))
    out = np.zeros((4, 4096, D), np.float32)
    for r in range(NCORES):
        b, half = r // 2, r % 2
        out[b, half * T:(half + 1) * T, :] = np.asarray(res.results[r]["y"], dtype=np.float32)
    return out
```

```python
import contextlib
import os
DBG_EX = os.environ.get('DBG_EX', '')
DBG_MB = int(os.environ.get('DBG_MB', '99'))
DBG_G = os.environ.get('DBG_G', '')
DBG_H = os.environ.get('DBG_H', '')
DBG_Y = os.environ.get('DBG_Y', '')
DBG_STOP = os.environ.get('DBG_STOP', 'final')
import numpy as np
import concourse.bass as bass
import concourse.mybir as mybir
from concourse.bass_utils import run_bass_kernel_spmd

F32 = mybir.dt.float32
BF16 = mybir.dt.bfloat16
AF = mybir.ActivationFunctionType
ALU = mybir.AluOpType
AX = mybir.AxisListType

NCORES = 8
D = 1024
T = 2048
NT = 16
TB = 256
NBLK = T // TB
HALO = 32
EXT = HALO + TB
D_IN = 2832
D_FF = 2816
NE = 8
EPS = 1e-6
O_SCB, O_SCC, O_SCV, O_CFA, O_CFG, O_Q, O_K, O_V, O_ALR, O_R = 0, 256, 512, 768, 1024, 1280, 1536, 1792, 2304, 2320
FF_GROUPS = [(0, 4), (4, 4), (8, 4), (12, 4), (16, 4), (20, 2)]
PV_SCW, PV_CFW, PV_CFB, PV_LNG, PV_LNB, PV_BA, PV_N = 0, 6, 68, 70, 72, 74, 76
C_ID, C_ONES, C_TRI, C_SM, C_N = 0, 128, 256, 320, 576


class DSem:
    def __init__(self, name):
        self.name = name
        self.count = 0
        self.handle = None


class Op:
    __slots__ = ("eng", "fn", "waits", "mark", "semval", "idx", "dma")

    def __init__(self, eng, fn, dma):
        self.eng, self.fn, self.dma = eng, fn, dma
        self.waits = []
        self.mark = False
        self.semval = None
        self.idx = 0


class Sched:
    ENGS = ["pe", "act", "dve", "pool", "sp"]

    def __init__(self):
        self.ops = {e: [] for e in self.ENGS}
        self.res = {}
        self.pending = {e: [] for e in self.ENGS}
        self.dsems = []

    def dsem(self, name):
        s = DSem(name)
        self.dsems.append(s)
        return s

    def add(self, eng, fn, r=(), w=(), dma=None, rg=None):
        op = Op(eng, fn, dma)
        op.idx = len(self.ops[eng])
        deps = []
        force = None
        if eng == "pe":
            prev = getattr(self, "_pe_rg", None)
            if rg is not None and prev is not None and (rg[0] + rg[1] <= prev[0] or prev[0] + prev[1] <= rg[0]):
                force = self.ops["pe"][-1]
            self._pe_rg = rg
        for x in r:
            st = self.res.get(x)
            if st is not None and st[0] is not None:
                deps.append(st[0])
        for x in w:
            st = self.res.get(x)
            if st is not None:
                if st[0] is not None:
                    deps.append(st[0])
                deps.extend(st[1])
        deps.extend(self.pending[eng])
        self.pending[eng] = []
        if force is not None:
            force.mark = True
            op.waits.append(("op", force))
        seen = set()
        for tok in deps:
            if id(tok) in seen:
                continue
            seen.add(id(tok))
            if tok[0] == "op":
                p = tok[1]
                if p.eng == eng:
                    if eng == "pe":
                        continue
                    if dma is None and (op.idx - p.idx) > 2:
                        continue
                p.mark = True
            op.waits.append(tok)
        if dma is not None:
            dma.count += 1
            tok = ("dma", dma, dma.count * 16)
        else:
            tok = ("op", op)
        for x in r:
            st = self.res.setdefault(x, [None, []])
            st[1].append(tok)
        for x in w:
            self.res[x] = [tok, []]
        self.ops[eng].append(op)
        return op

    def barrier(self):
        toks = []
        for e in self.ENGS:
            if self.ops[e]:
                last = None
                for o in reversed(self.ops[e]):
                    if o.dma is None and o.fn is not None:
                        last = o
                        break
                if last is not None:
                    last.mark = True
                    toks.append(("op", last))
        for s in self.dsems:
            if s.count:
                toks.append(("dma", s, s.count * 16))
        for e in self.ENGS:
            self.pending[e] = [t for t in toks if not (t[0] == "op" and t[1].eng == e and e == "pe")]
        self.res = {}

    def emit(self, nc, stack):
        EPOCH = 30000
        engsems = {}
        for e in self.ENGS:
            n = 0
            for o in self.ops[e]:
                if o.mark:
                    n += 1
                    o.semval = n
            nep = max(1, (n + EPOCH - 1) // EPOCH)
            engsems[e] = [stack.enter_context(nc.semaphore(f"s_{e}{i}")) for i in range(nep)]
        for s in self.dsems:
            if s.count:
                s.handle = stack.enter_context(nc.semaphore("d_" + s.name))

        def semof(e, val):
            i = (val - 1) // EPOCH
            return engsems[e][i], val - i * EPOCH

        block = stack.enter_context(nc.Block())

        def run(ename):
            def body(eng):
                waited = {}
                for o in self.ops[ename]:
                    for tok in o.waits:
                        if tok[0] == "op":
                            sem, val = semof(tok[1].eng, tok[1].semval)
                        else:
                            sem, val = tok[1].handle, tok[2]
                        key = id(sem)
                        if waited.get(key, 0) >= val:
                            continue
                        waited[key] = val
                        eng.wait_ge(sem, val)
                    if o.fn is None:
                        continue
                    ins = o.fn(eng)
                    if o.dma is not None:
                        ins.then_inc(o.dma.handle, 16)
                    elif o.mark:
                        sem, _ = semof(ename, o.semval)
                        ins.then_inc(sem, 1)
            return body

        block.tensor(run("pe"))
        block.scalar(run("act"))
        block.vector(run("dve"))
        block.gpsimd(run("pool"))
        block.sync(run("sp"))


STAGES = ['load', 'mixA', 'ffnA', 'preA', 'mix0', 'ffn0', 'mix1', 'ffn1', 'final']


def build_program(stop='final'):
    nc = bass.Bass("TRN2", target_bir_lowering=False)
    S = Sched()
    SI = STAGES.index(stop)
    need_moe = SI >= STAGES.index('ffn1')

    def din(name, shape):
        return nc.dram_tensor(name, list(shape), F32, kind="ExternalInput").ap()

    x_d = din("x", [T, D])
    xp_d = din("x_prev", [T, D])
    sel_d = din("sel", [128, NCORES])
    consts_d = din("consts", [128, C_N])
    pvec_d = din("pvec", [2, 128, PV_N])
    bvec_d = din("bvec", [5, 128, D])
    gng_d = din("gng", [2, 128, 512])
    wa2_d = din("w_a2", [2, 16, 256])
    win_d = din("w_in", [2, D, D_IN])
    wout_d = din("w_out", [2, D, D])
    dwg_d = din("dense_w_gate", [1, D, D_FF])
    dwu_d = din("dense_w_up", [1, D, D_FF])
    dwd_d = din("dense_w_down", [1, D_FF, D])
    wr_d = din("moe_w_router", [1, D, NE])
    if need_moe:
        mwg_d = din("moe_w_gate", [1, NE, D, D_FF])
        mwu_d = din("moe_w_up", [1, NE, D, D_FF])
        mwd_d = din("moe_w_down", [1, NE, D_FF, D])
    y_d = nc.dram_tensor("y", [T, D], F32, kind="ExternalOutput").ap()

    stack = contextlib.ExitStack()
    with stack:
        def sb(name, shape, dt=F32):
            return stack.enter_context(nc.sbuf_tensor("sb_" + name, list(shape), dt))

        h = sb("h", [128, NT, D])
        wbuf = sb("wbuf", [128, 8 * D_IN + 8 * D], BF16)
        W_in = wbuf[:, 0:8 * D_IN].rearrange("p (k c) -> p k c", k=8)
        W_out = wbuf[:, 8 * D_IN:8 * D_IN + 8 * D].rearrange("p (k c) -> p k c", k=8)
        FW = []
        for s_ in range(2):
            base = s_ * 12288
            wg = wbuf[:, base:base + 4096].rearrange("p (k c) -> p k c", k=8)
            wu = wbuf[:, base + 4096:base + 8192].rearrange("p (k c) -> p k c", k=8)
            wd = wbuf[:, base + 8192:base + 12288].rearrange("p (g c) -> p g c", g=4)
            FW.append((wg, wu, wd))
        RR_N = 9088
        RR = sb("RR", [128, RR_N], F32)
        fT = RR[:, 0:8192].bitcast(BF16).rearrange("p (k t) -> p k t", k=8)
        _off = [0]

        def carve(n_f32, dt=F32, shape=None):
            a = RR[:, _off[0]:_off[0] + n_f32]
            _off[0] += n_f32
            if dt == BF16:
                a = a.bitcast(BF16)
            return a

        S_A = carve(256).rearrange("p (a t) -> p a t", a=2)
        S_T = carve(256).rearrange("p (a t) -> p a t", a=2)
        S_bf = carve(128, BF16).rearrange("p (a t) -> p a t", a=2)
        _x0 = _off[0]
        csp = carve(512).rearrange("p (a t) -> p a t", a=2)
        E_ = carve(512).rearrange("p (a t) -> p a t", a=2)
        Ei = carve(512).rearrange("p (a t) -> p a t", a=2)
        qtT = carve(256, BF16).rearrange("p (a t) -> p a t", a=2)
        ktT = carve(256, BF16).rearrange("p (a t) -> p a t", a=2)
        kt = carve(256, BF16).rearrange("p (a t) -> p a t", a=2)
        vv = carve(512, BF16).rearrange("p (a t) -> p a t", a=2)
        sr = carve(512, BF16).rearrange("p (a t) -> p a t", a=2)
        m_ = carve(2 * EXT).rearrange("p (a t) -> p a t", a=2)
        u_ = carve(EXT, BF16).rearrange("p (a t) -> p a t", a=2)
        tmpx = carve(EXT)
        acc_sc = carve(256)
        cfc = carve(512).rearrange("p (a t) -> p a t", a=2)
        cfsq = carve(512).rearrange("p (a t) -> p a t", a=2)
        mean_sb = carve(256)
        var_sb = carve(256)
        rstd_sb = carve(256)
        yT = carve(1024, BF16).rearrange("p (a t) -> p a t", a=8)
        og = carve(256, BF16)
        ogb = carve(256, BF16)
        AT = carve(128, BF16).rearrange("p (a t) -> p a t", a=4)
        alrT = carve(256)
        assert _off[0] <= RR_N, _off[0]
        _off[0] = _x0
        stS = carve(256)
        stH = carve(1024)
        haloh = carve(1024)
        outst = RR[:, 0:2048].rearrange("p (a t) -> p a t", a=2)

        aTx = sb("aTx", [128, 8, EXT], BF16)
        FR = sb("FR", [128, 6656], F32)
        actT = FR[:, 0:2048].bitcast(BF16).rearrange("p (a j t) -> p a j t", a=2, j=4)
        f32t = FR[:, 2048:3072]
        f32T = FR[:, 3072:4096].rearrange("p (k t) -> p k t", k=8)
        sg = FR[:, 4096:4608].bitcast(BF16).rearrange("p (a t) -> p a t", a=2)
        D_cf = FR[:, 0:3968].bitcast(BF16).rearrange("p (j c) -> p j c", j=62)
        aTx1 = FR[:, 3968:5120].bitcast(BF16).rearrange("p (k t) -> p k t", k=8)
        csp1 = FR[:, 5120:5632].rearrange("p (a t) -> p a t", a=2)
        E_1 = FR[:, 5632:6144].rearrange("p (a t) -> p a t", a=2)
        Ei1 = FR[:, 6144:6656].rearrange("p (a t) -> p a t", a=2)
        a_bf = sb("a_bf", [128, D], BF16)
        gvec = sb("gvec", [128, D])
        consts = sb("consts", [128, C_N])
        ident_bf = sb("ident_bf", [128, 128], BF16)
        pvec = sb("pvec", [128, 2, PV_N])
        nba = sb("nba", [128, 2, 2])
        gng = sb("gng", [128, 512])
        wa2 = sb("wa2", [16, 2, 256])
        wr = sb("wr", [128, 8, NE])
        sel = sb("sel", [128, NCORES])
        small = sb("small", [128, 64])
        Ssave = sb("Ssave", [128, 2, 256])
        Hsave = sb("Hsave", [128, 2, 8 * HALO], BF16)
        gates = sb("gates", [128, NT, NE])

        psF = [stack.enter_context(nc.psum_tensor(f"psF{i}", [128, 512], F32)) for i in range(6)]
        psB = [stack.enter_context(nc.psum_tensor(f"psB{i}", [128, 1024], BF16)) for i in range(2)]
        _rr = [0, 0]

        def nextF():
            i = _rr[0] % 5
            _rr[0] += 1
            return psF[i], f"F{i}"

        def nextB():
            i = _rr[1] % 2
            _rr[1] += 1
            return psB[i], f"B{i}"

        ident = consts[:, C_ID:C_ID + 128]
        ones256 = consts[:, C_ONES:C_ONES + 128]
        tri = consts[:, C_TRI:C_TRI + 64]
        smask = consts[:, C_SM:C_SM + 256]

        def mm(out, lhsT, rhs, start, stop, r, w):
            rg = (lhsT.base_partition(), lhsT.partition_size())
            S.add("pe", lambda e: e.matmul(out, lhsT=lhsT, rhs=rhs, start=start, stop=stop), r=r, w=w, rg=rg)

        def tr(out, in_, idn, r, w):
            rg = (in_.base_partition(), in_.partition_size())
            S.add("pe", lambda e: e.transpose(out=out, in_=in_, identity=idn), r=r, w=w, rg=rg)

        def act(out, in_, func, r, w, bias=None, scale=None, accum=None):
            kw = {}
            if bias is not None:
                kw["bias"] = bias
            if scale is not None:
                kw["scale"] = scale
            if accum is not None:
                kw["accum_out"] = accum
            S.add("act", lambda e: e.activation(out=out, in_=in_, func=func, **kw), r=r, w=w)

        def tt(out, in0, in1, op, r, w, eng="dve"):
            S.add(eng, lambda e: e.tensor_tensor(out=out, in0=in0, in1=in1, op=op), r=r, w=w)

        def ts(out, in0, s1, op0, r, w, s2=None, op1=None, eng="dve"):
            if op1 is None:
                S.add(eng, lambda e: e.tensor_scalar(out=out, in0=in0, scalar1=s1, scalar2=None, op0=op0), r=r, w=w)
            else:
                S.add(eng, lambda e: e.tensor_scalar(out=out, in0=in0, scalar1=s1, scalar2=s2, op0=op0, op1=op1), r=r, w=w)

        def stt(out, in0, scalar, in1, op0, op1, r, w):
            S.add("dve", lambda e: e.scalar_tensor_tensor(out=out, in0=in0, scalar=scalar, in1=in1, op0=op0, op1=op1), r=r, w=w)

        def cp(out, in_, r, w, eng="dve"):
            S.add(eng, lambda e: e.tensor_copy(out=out, in_=in_), r=r, w=w)

        def dma(q, out, in_, r, w, sem):
            S.add(q, lambda e: e.dma_start(out=out, in_=in_), r=r, w=w, dma=sem)

        s_misc = [S.dsem(f"misc{i}") for i in range(10)]
        dma("sp", consts[:], consts_d, [], ["consts"], s_misc[0])
        dma("pool", ident_bf[:], consts_d[:, C_ID:C_ID + 128], [], ["ident_bf"], s_misc[1])
        dma("sp", pvec[:], pvec_d.rearrange("l p n -> p l n"), [], ["pvec"], s_misc[2])
        dma("sp", wa2[:], wa2_d.rearrange("l r n -> r l n"), [], ["wa2"], s_misc[4])
        dma("sp", wr[:], wr_d[0].rearrange("(k p) e -> p k e", p=128), [], ["wr"], s_misc[5])
        dma("sp", sel[:], sel_d, [], ["sel"], s_misc[6])
        s_x = [S.dsem(f"x{t}") for t in range(NT)]

        def load_x(src):
            for t in range(NT):
                dma("sp", h[:, t, :], src[t * 128:(t + 1) * 128, :], [], [f"h{t}"], s_x[t])
        ts(nba[:], pvec[:, :, PV_BA:PV_BA + 2], -1.0, ALU.mult, ["pvec"], ["nba"])

        s_win = [S.dsem(f"win{k}") for k in range(8)]
        s_wout = S.dsem("wout")
        s_gv = S.dsem("gv")
        s_fw = [[S.dsem(f"fw{s_}{j}") for j in range(3)] for s_ in range(2)]

        def load_mixer_weights(l):
            for k in range(8):
                dma("pool", W_in[:, k, :], win_d[l, k * 128:(k + 1) * 128, :], [], [f"Win{k}"] + [f"fw{s_}{j}" for s_ in range(2) for j in range(3)], s_win[k])
            dma("pool", W_out[:], wout_d[l].rearrange("(k p) c -> p k c", p=128), [], ["Wout"], s_wout)

        def norm_tile(src, src_res, nrows, gidx_res, dstT, dst_res, want_f32=False):
            ss = small[0:nrows, 0:1]
            lnv = small[0:nrows, 1:2]
            rstd = small[0:nrows, 2:3]
            act(a_bf[0:nrows, :], src, AF.Square, [src_res], ["a_bf", "ss"], accum=ss)
            act(lnv, ss, AF.Ln, ["ss"], ["lnv"], bias=EPS, scale=1.0 / D)
            act(rstd, lnv, AF.Exp, ["lnv"], ["rstd"], scale=-0.5)
            if want_f32:
                stt(f32t[0:nrows, :], src, rstd, gvec[0:nrows, :], ALU.mult, ALU.mult, [src_res, "rstd", gidx_res], ["f32t"])
                act(a_bf[0:nrows, :], f32t[0:nrows, :], AF.Copy, ["f32t"], ["a_bf"])
            else:
                stt(a_bf[0:nrows, :], src, rstd, gvec[0:nrows, :], ALU.mult, ALU.mult, [src_res, "rstd", gidx_res], ["a_bf"])
            pb, pbn = nextB()
            for k in range(8):
                tr(pb[:, k * nrows:(k + 1) * nrows], a_bf[0:nrows, k * 128:(k + 1) * 128], ident_bf[0:nrows, 0:nrows],
                   ["a_bf", "ident_bf"], [pbn])
            cp(dstT, pb[:, 0:8 * nrows].rearrange("p (k t) -> p k t", k=8), [pbn], [dst_res], eng="act" if False else "dve")

        def load_gvec(i):
            dma("sp", gvec[:], bvec_d[i], [], ["gvec"], s_gv)

        class BS:
            pass

        PB = []
        for i_, (a_, c_, e_, ei_) in enumerate([(aTx, csp, E_, Ei), (aTx1, csp1, E_1, Ei1)]):
            o_ = BS()
            o_.aTx, o_.csp, o_.E, o_.Ei = a_, c_, e_, ei_
            o_.n_aTx, o_.n_csp, o_.n_E, o_.n_Ei = f"aTx{i_}", f"csp{i_}", f"E{i_}", f"Ei{i_}"
            PB.append(o_)

        def proj_fm(col0, ncols, rhs, rhs_res, N):
            p, pn = nextF()
            for k in range(8):
                mm(p[0:ncols, 0:N], W_in[:, k, col0:col0 + ncols], rhs[:, k, :], k == 0, k == 7, [f"Win{k}", rhs_res], [pn])
            return p, pn

        def prep_stream(l, b, P, Pprev):
            if Pprev is not None:
                cp(P.aTx[:, :, 0:HALO], Pprev.aTx[:, :, EXT - HALO:EXT], [Pprev.n_aTx], [P.n_aTx])
            for ti in range(2):
                t = b * 2 + ti
                norm_tile(h[:, t, :], f"h{t}", 128, "gvec", P.aTx[:, :, HALO + ti * 128:HALO + (ti + 1) * 128], P.n_aTx)
                yield
            rhs = P.aTx[:, :, HALO:EXT]
            p, pn = proj_fm(O_ALR, 16, rhs, P.n_aTx, TB)
            cp(alrT[0:16, :], p[0:16, 0:TB], [pn], ["alrT"])
            yield
            pz, pzn = nextF()
            for hp in range(2):
                mm(pz[:, hp * 256:(hp + 1) * 256], wa2[0:16, l, hp * 128:(hp + 1) * 128], alrT[0:16, :], True, True, ["wa2", "alrT"], [pzn])
            for hp in range(2):
                act(P.csp[:, hp, :], pz[:, hp * 256:(hp + 1) * 256], AF.Exp, [pzn, "nba"], [P.n_csp], bias=nba[:, l, hp:hp + 1], scale=-1.0)
            yield
            act(P.csp[:], P.csp[:], AF.Ln, [P.n_csp], [P.n_csp], bias=1.0)
            for hp in range(2):
                S.add("dve", (lambda hp_: (lambda e: e.tensor_tensor_scan(out=P.csp[:, hp_, :], data0=smask, data1=P.csp[:, hp_, :], initial=0.0, op0=ALU.mult, op1=ALU.add)))(hp),
                      r=[P.n_csp, "consts"], w=[P.n_csp])
            yield
            act(P.Ei[:], P.csp[:], AF.Exp, [P.n_csp], [P.n_Ei], scale=1.0 / 16.0)
            act(P.E[:], P.csp[:], AF.Exp, [P.n_csp], [P.n_E], scale=-1.0 / 16.0)
            yield

        def kv_block(P):
            rhs = P.aTx[:, :, HALO:EXT]
            pk, pkn = nextF()
            for hp in range(2):
                for k in range(8):
                    mm(pk[:, hp * 256:(hp + 1) * 256], W_in[:, k, O_K + hp * 128:O_K + (hp + 1) * 128], rhs[:, k, :], k == 0, k == 7, [f"Win{k}", P.n_aTx], [pkn])
            tt(ktT[:], pk[:].rearrange("p (a t) -> p a t", a=2), P.Ei[:], ALU.mult, [pkn, P.n_Ei], ["ktT"])
            pb, pbn = nextB()
            for ti in range(2):
                for hp in range(2):
                    tr(pb[:, ti * 256 + hp * 128: ti * 256 + (hp + 1) * 128], ktT[:, hp, ti * 128:(ti + 1) * 128], ident_bf[:], ["ktT", "ident_bf"], [pbn])
            cp(kt[:], pb[:, 0:512].rearrange("p (a t) -> p a t", a=2), [pbn], ["kt"])
            for ti in range(2):
                pv, pvn = nextF()
                for k in range(8):
                    mm(pv[:], P.aTx[:, k, HALO + ti * 128:HALO + (ti + 1) * 128], W_in[:, k, O_V:O_V + 512], k == 0, k == 7, [f"Win{k}", P.n_aTx], [pvn])
                act(vv[:, ti, :], pv[:], AF.Copy, [pvn], ["vv"])

        def state_update(c, P):
            ti, par = c // 2, c % 2
            rows = slice(par * 64, par * 64 + 64)
            pS, pSn = nextF()
            for hh in range(4):
                hp, hl = hh // 2, hh % 2
                mm(pS[hl * 64:(hl + 1) * 64, hp * 128:(hp + 1) * 128], kt[rows, ti, hh * 64:(hh + 1) * 64], vv[rows, ti, hh * 128:(hh + 1) * 128],
                   True, True, ["kt", "vv"], [pSn])
            tt(S_T[:], pS[:, 0:256].rearrange("p (a t) -> p a t", a=2), S_A[:], ALU.add, [pSn, "S_A"], ["S_T"])
            for hp in range(2):
                ts(S_A[:, hp, :], S_T[:, hp, :], P.E[:, hp, c * 64 + 63:c * 64 + 64], ALU.mult, ["S_T", P.n_E], ["S_A"])
            cp(S_bf[:], S_A[:], ["S_A"], ["S_bf"])

        def prepass_stream(P):
            kv_block(P)
            yield
            for c in range(4):
                state_update(c, P)
                yield

        def interleave(*gens):
            gens = [g_ for g_ in gens if g_ is not None]
            while gens:
                for g_ in list(gens):
                    try:
                        next(g_)
                    except StopIteration:
                        gens.remove(g_)

        def conv_stream(l, b, P):
            aX, nX = P.aTx, P.n_aTx
            rhs = aX[:, :, HALO:EXT]
            for cc in range(2):
                pc, pcn = nextF()
                for k in range(8):
                    mm(pc[:, 0:EXT], W_in[:, k, O_SCC + cc * 128:O_SCC + (cc + 1) * 128], aX[:, k, :], k == 0, k == 7, [f"Win{k}", nX], [pcn])
                pv, pvn = nextF()
                for k in range(8):
                    mm(pv[:, 0:EXT], W_in[:, k, O_SCV + cc * 128:O_SCV + (cc + 1) * 128], aX[:, k, :], k == 0, k == 7, [f"Win{k}", nX], [pvn])
                act(tmpx[:], pc[:, 0:EXT], AF.Copy, [pcn], ["tmpx"])
                tt(m_[:, cc, :], tmpx[:], pv[:, 0:EXT], ALU.mult, ["tmpx", pvn], ["m"])
                yield
                w0 = pvec[:, l, PV_SCW + cc * 3 + 0:PV_SCW + cc * 3 + 1]
                w1 = pvec[:, l, PV_SCW + cc * 3 + 1:PV_SCW + cc * 3 + 2]
                w2 = pvec[:, l, PV_SCW + cc * 3 + 2:PV_SCW + cc * 3 + 3]
                ts(acc_sc[:], m_[:, cc, HALO - 2:HALO - 2 + TB], w0, ALU.mult, ["m", "pvec"], ["acc_sc"])
                stt(acc_sc[:], m_[:, cc, HALO - 1:HALO - 1 + TB], w1, acc_sc[:], ALU.mult, ALU.add, ["m", "pvec", "acc_sc"], ["acc_sc"])
                stt(acc_sc[:], m_[:, cc, HALO:HALO + TB], w2, acc_sc[:], ALU.mult, ALU.add, ["m", "pvec", "acc_sc"], ["acc_sc"])
                pb_, pbn_ = proj_fm(O_SCB + cc * 128, 128, rhs, nX, TB)
                tt(yT[:, cc, :], acc_sc[:], pb_[:, 0:TB], ALU.mult, ["acc_sc", pbn_], ["yT_sc"])
                yield
            for cc in range(2):
                pa, pan = nextF()
                for k in range(8):
                    mm(pa[:, 0:EXT], W_in[:, k, O_CFA + cc * 128:O_CFA + (cc + 1) * 128], aX[:, k, :], k == 0, k == 7, [f"Win{k}", nX], [pan])
                pg, pgn = nextF()
                for k in range(8):
                    mm(pg[:, 0:EXT], W_in[:, k, O_CFG + cc * 128:O_CFG + (cc + 1) * 128], aX[:, k, :], k == 0, k == 7, [f"Win{k}", nX], [pgn])
                act(tmpx[:], pg[:, 0:EXT], AF.Tanh, [pgn], ["tmpx"], scale=0.5)
                stt(u_[:, cc, :], tmpx[:], 1.0, pa[:, 0:EXT], ALU.add, ALU.mult, ["tmpx", pan], ["u"])
                yield
                pcv, pcvn = nextF()
                for kk in range(31):
                    mm(pcv[:, 0:TB], D_cf[:, cc * 31 + kk, :], u_[:, cc, HALO - 30 + kk:HALO - 30 + kk + TB], kk == 0, kk == 30, ["Dcf", "u"], [pcvn])
                act(cfc[:, cc, :], pcv[:, 0:TB], AF.Identity, [pcvn, "pvec"], ["cfc"], bias=pvec[:, l, PV_CFB + cc:PV_CFB + cc + 1], scale=0.5)
                act(cfsq[:, cc, :], cfc[:, cc, :], AF.Square, ["cfc"], ["cfsq"])
                yield
            pm, pmn = nextF()
            for cc in range(2):
                mm(pm[:, 0:TB], ones256, cfc[:, cc, :], cc == 0, cc == 1, ["consts", "cfc"], [pmn])
            for cc in range(2):
                mm(pm[:, TB:2 * TB], ones256, cfsq[:, cc, :], cc == 0, cc == 1, ["consts", "cfsq"], [pmn])
            cp(mean_sb[:], pm[:, 0:TB], [pmn], ["mean"])
            tt(var_sb[:], mean_sb[:], mean_sb[:], ALU.mult, ["mean"], ["var"])
            tt(var_sb[:], pm[:, TB:2 * TB], var_sb[:], ALU.subtract, [pmn, "var"], ["var"])
            yield
            act(rstd_sb[:], var_sb[:], AF.Ln, ["var"], ["rstdc"], bias=EPS)
            act(rstd_sb[:], rstd_sb[:], AF.Exp, ["rstdc"], ["rstdc"], scale=-0.5)
            for cc in range(2):
                tt(cfc[:, cc, :], cfc[:, cc, :], mean_sb[:], ALU.subtract, ["cfc", "mean"], ["cfc"])
                tt(cfc[:, cc, :], cfc[:, cc, :], rstd_sb[:], ALU.mult, ["cfc", "rstdc"], ["cfc"])
                act(yT[:, 2 + cc, :], cfc[:, cc, :], AF.Silu, ["cfc", "pvec"], ["yT_cf"],
                    bias=pvec[:, l, PV_LNB + cc:PV_LNB + cc + 1], scale=pvec[:, l, PV_LNG + cc:PV_LNG + cc + 1])
                yield

        def gla_stream(l, b, P):
            aX, nX = P.aTx, P.n_aTx
            rhs = aX[:, :, HALO:EXT]
            pq, pqn = nextF()
            for hp in range(2):
                for k in range(8):
                    mm(pq[:, hp * 256:(hp + 1) * 256], W_in[:, k, O_Q + hp * 128:O_Q + (hp + 1) * 128], rhs[:, k, :], k == 0, k == 7, [f"Win{k}", nX], [pqn])
            stt(qtT[:], pq[:].rearrange("p (a t) -> p a t", a=2), 0.125, P.E[:], ALU.mult, ALU.mult, [pqn, P.n_E], ["qtT"])
            yield
            kv_block(P)
            yield
            for ti in range(2):
                pr, prn = nextF()
                for k in range(8):
                    mm(pr[:], aX[:, k, HALO + ti * 128:HALO + (ti + 1) * 128], W_in[:, k, O_R:O_R + 512], k == 0, k == 7, [f"Win{k}", nX], [prn])
                act(sr[:, ti, :], pr[:], AF.Silu, [prn], ["sr"])
                tt(sr[:, ti, :], sr[:, ti, :], gng[:, :], ALU.mult, ["sr", "gng"], ["sr"])
                yield
            po, pon = psF[5], "F5"
            for ti in range(2):
                for par in range(2):
                    c = ti * 2 + par
                    rows = slice(par * 64, par * 64 + 64)
                    cols = slice(c * 64, c * 64 + 64)
                    pA, pAn = nextF()
                    for hh in (0, 2, 1, 3):
                        hp, hl = hh // 2, hh % 2
                        fr = slice(hl * 64, hl * 64 + 64)
                        mm(pA[rows, hh * 64:(hh + 1) * 64], ktT[fr, hp, cols], qtT[fr, hp, cols], True, True, ["ktT", "qtT"], [pAn])
                    tt(AT[rows, :, :], pA[rows, 0:256].rearrange("p (a t) -> p a t", a=4), tri[rows, :].unsqueeze(1).to_broadcast([64, 4, 64]),
                       ALU.mult, [pAn, "consts"], ["AT"])
                    yield
                    for hh in ((0, 2, 1, 3) if par == 0 else (1, 3, 0, 2)):
                        hp, hl = hh // 2, hh % 2
                        fr = slice(hl * 64, hl * 64 + 64)
                        mm(po[rows, hh * 128:(hh + 1) * 128], qtT[fr, hp, cols], S_bf[fr, hp, :], True, False, ["qtT", "S_bf"], [pon])
                        mm(po[rows, hh * 128:(hh + 1) * 128], AT[rows, hh, :], vv[rows, ti, hh * 128:(hh + 1) * 128], False, True, ["AT", "vv"], [pon])
                    yield
                    state_update(c, P)
                    yield
                for hh in range(4):
                    act(og[:, hh * 128:(hh + 1) * 128], po[:, hh * 128:(hh + 1) * 128], AF.Square, [pon], ["og", "ss4"], accum=small[:, 8 + hh:9 + hh])
                act(small[:, 12:16], small[:, 8:12], AF.Ln, ["ss4"], ["ln4"], bias=EPS, scale=1.0 / 128.0)
                act(small[:, 16:20], small[:, 12:16], AF.Exp, ["ln4"], ["rstd4"], scale=-0.5)
                yield
                for hh in range(4):
                    stt(ogb[:, hh * 128:(hh + 1) * 128], po[:, hh * 128:(hh + 1) * 128], small[:, 16 + hh:17 + hh], sr[:, ti, hh * 128:(hh + 1) * 128],
                        ALU.mult, ALU.mult, [pon, "rstd4", "sr"], ["ogb"])
                pb, pbn = nextB()
                for j in range(4):
                    tr(pb[:, j * 128:(j + 1) * 128], ogb[:, j * 128:(j + 1) * 128], ident_bf[:], ["ogb", "ident_bf"], [pbn])
                cp(yT[:, 4:8, ti * 128:(ti + 1) * 128], pb[:, 0:512].rearrange("p (a t) -> p a t", a=4), [pbn], ["yT_gla"])
                yield

        def wout_block(b):
            for ti in range(2):
                t = b * 2 + ti
                for half in range(2):
                    pw, pwn = nextF()
                    for k in range(8):
                        mm(pw[:], yT[:, k, ti * 128:(ti + 1) * 128], W_out[:, k, half * 512:(half + 1) * 512], k == 0, k == 7,
                           ["yT_sc", "yT_cf", "yT_gla", "Wout"], [pwn])
                    tt(h[:, t, half * 512:(half + 1) * 512], pw[:], h[:, t, half * 512:(half + 1) * 512], ALU.add, [pwn, f"h{t}"], [f"h{t}"])

        def save_state(i):
            P = PB[(NBLK - 1) % 2]
            ts(Ssave[:, i, :], S_A[:].rearrange("p a t -> p (a t)"), sel[:, 0:1], ALU.mult, ["S_A", "sel"], [f"Ssave{i}"])
            ts(Hsave[:, i, :].rearrange("p (k t) -> p k t", k=8), P.aTx[:, :, EXT - HALO:EXT], sel[:, 0:1], ALU.mult, [P.n_aTx, "sel"], [f"Hsave{i}"])

        def mixer_main(l, src):
            load_gvec(l)
            load_mixer_weights(l)
            dma("sp", gng[:], gng_d[l], [], ["gng"], s_misc[3])
            if src is None:
                S.add("dve", lambda e: e.memset(S_A[:], 0.0), r=[], w=["S_A"])
                S.add("dve", lambda e: e.memset(PB[0].aTx[:, :, 0:HALO], 0.0), r=[], w=[PB[0].n_aTx])
            else:
                cp(S_A[:].rearrange("p a t -> p (a t)"), Ssave[:, src, :], [f"Ssave{src}"], ["S_A"])
                cp(PB[0].aTx[:, :, 0:HALO], Hsave[:, src, :].rearrange("p (k t) -> p k t", k=8), [f"Hsave{src}"], [PB[0].n_aTx])
            cp(S_bf[:], S_A[:], ["S_A"], ["S_bf"])
            for j in range(62):
                ts(D_cf[:, j, :], ident_bf[:], pvec[:, l, PV_CFW + j:PV_CFW + j + 1], ALU.mult, ["ident_bf", "pvec"], ["Dcf"])
            interleave(prep_stream(l, 0, PB[0], None))
            for b in range(NBLK):
                P = PB[b % 2]
                nxt = prep_stream(l, b + 1, PB[(b + 1) % 2], P) if b + 1 < NBLK else None
                interleave(gla_stream(l, b, P), conv_stream(l, b, P), nxt)
                wout_block(b)

        def mixer_prepass(l):
            load_gvec(l)
            load_mixer_weights(l)
            S.add("dve", lambda e: e.memset(S_A[:], 0.0), r=[], w=["S_A"])
            interleave(prep_stream(l, 0, PB[0], None))
            for b in range(NBLK):
                P = PB[b % 2]
                nxt = prep_stream(l, b + 1, PB[(b + 1) % 2], P) if b + 1 < NBLK else None
                interleave(prepass_stream(P), nxt)

        def ffn_norm_tiles(moe, tiles):
            for t in tiles:
                norm_tile(h[:, t, :], f"h{t}", 128, "gvec", fT[:, :, t * 128:(t + 1) * 128], f"fT{t // 4}", want_f32=moe)
                if moe:
                    router_tile(t)

        def router_tile(t):
            pl, pln = nextF()
            for half in range(2):
                pt, ptn = nextF()
                for j in range(4):
                    k = half * 4 + j
                    tr(pt[:, j * 128:(j + 1) * 128], f32t[:, k * 128:(k + 1) * 128], ident, ["f32t", "consts"], [ptn])
                cp(f32T[:, half * 4:(half + 1) * 4, :], pt[:].rearrange("p (a t) -> p a t", a=4), [ptn], ["f32T"])
            for k in range(8):
                mm(pl[:, 0:NE], f32T[:, k, :], wr[:, k, :], k == 0, k == 7, ["f32T", "wr"], [pln])
            lg = small[:, 24:32]
            m1 = small[:, 32:33]
            m2 = small[:, 33:34]
            eq = small[:, 34:42]
            l2 = small[:, 42:50]
            ex = small[:, 50:58]
            den = small[:, 58:59]
            nm1 = small[:, 59:60]
            cp(lg, pl[:, 0:NE], [pln], ["lg"])
            S.add("dve", lambda e: e.reduce_max(out=m1, in_=lg, axis=AX.X), r=["lg"], w=["m1"])
            ts(eq, lg, m1, ALU.is_equal, ["lg", "m1"], ["eq"])
            stt(l2, eq, -1e30, lg, ALU.mult, ALU.add, ["eq", "lg"], ["l2"])
            S.add("dve", lambda e: e.reduce_max(out=m2, in_=l2, axis=AX.X), r=["l2"], w=["m2"])
            ts(eq, lg, m2, ALU.is_ge, ["lg", "m2"], ["eq"])
            ts(nm1, m1, -1.0, ALU.mult, ["m1"], ["nm1"])
            act(ex, lg, AF.Exp, ["lg", "nm1"], ["ex"], bias=nm1, scale=1.0)
            tt(ex, ex, eq, ALU.mult, ["ex", "eq"], ["ex"])
            S.add("dve", lambda e: e.reduce_sum(out=den, in_=ex, axis=AX.X), r=["ex"], w=["den"])
            S.add("dve", lambda e: e.reciprocal(out=den, in_=den), r=["den"], w=["den"])
            ts(gates[:, t, :], ex, den, ALU.mult, ["ex", "den"], ["gates"])

        _fwi = [0]

        def ffn_group(wg_src, wu_src, wd_src, c0, G, gate_col, pre_tg=None):
            s_ = _fwi[0] % 2
            _fwi[0] += 1
            wg, wu, wd = FW[s_]
            dma("pool", wg[:, :, 0:G * 128], wg_src.rearrange("(k p) c -> p k c", p=128)[:, :, c0 * 128:(c0 + G) * 128], [], [f"fw{s_}0"], s_fw[s_][0])
            dma("pool", wu[:, :, 0:G * 128], wu_src.rearrange("(k p) c -> p k c", p=128)[:, :, c0 * 128:(c0 + G) * 128], [], [f"fw{s_}1"], s_fw[s_][1])
            dma("pool", wd[:, 0:G, :], wd_src[c0 * 128:(c0 + G) * 128, :].rearrange("(g p) c -> p g c", p=128), [], [f"fw{s_}2"], s_fw[s_][2])
            for tg in range(4):
                ab = tg % 2
                if pre_tg is not None:
                    pre_tg(tg)
                for j in range(G):
                    pg, pgn = nextF()
                    for k in range(8):
                        mm(pg[:], wg[:, k, j * 128:(j + 1) * 128], fT[:, k, tg * 512:(tg + 1) * 512], k == 0, k == 7, [f"fw{s_}0", f"fT{tg}"], [pgn])
                    pu, pun = nextF()
                    for k in range(8):
                        mm(pu[:], wu[:, k, j * 128:(j + 1) * 128], fT[:, k, tg * 512:(tg + 1) * 512], k == 0, k == 7, [f"fw{s_}1", f"fT{tg}"], [pun])
                    act(sg[:, j % 2, :], pg[:], AF.Silu, [pgn], [f"sg{j % 2}"])
                    tt(actT[:, ab, j, :], sg[:, j % 2, :], pu[:], ALU.mult, [f"sg{j % 2}", pun], [f"actT{ab}"])
                for tt_ in range(4):
                    t = tg * 4 + tt_
                    for half in range(2):
                        pd, pdn = nextF()
                        for j in range(G):
                            mm(pd[:], actT[:, ab, j, tt_ * 128:(tt_ + 1) * 128], wd[:, j, half * 512:(half + 1) * 512], j == 0, j == G - 1, [f"actT{ab}", f"fw{s_}2"], [pdn])
                        hs = h[:, t, half * 512:(half + 1) * 512]
                        if gate_col is None:
                            tt(hs, pd[:], hs, ALU.add, [pdn, f"h{t}"], [f"h{t}"])
                        else:
                            stt(hs, pd[:], gates[:, t, gate_col:gate_col + 1], hs, ALU.mult, ALU.add, [pdn, "gates", f"h{t}"], [f"h{t}"])

        def ffn(l):
            load_gvec(2 + l)
            moe = (l == 1)
            first = [True]

            def pre(tg):
                ffn_norm_tiles(moe, range(4 * tg, 4 * tg + 4))

            if not moe:
                for (c0, G) in FF_GROUPS:
                    ffn_group(dwg_d[0], dwu_d[0], dwd_d[0], c0, G, None, pre_tg=pre if first[0] else None)
                    first[0] = False
            else:
                for e_ in range(NE):
                    for (c0, G) in FF_GROUPS:
                        ffn_group(mwg_d[0, e_], mwu_d[0, e_], mwd_d[0, e_], c0, G, e_, pre_tg=pre if first[0] else None)
                        first[0] = False

        s_out = [S.dsem(f"out{i}") for i in range(2)]

        def final():
            load_gvec(4)
            for t in range(NT):
                i = t % 2
                ss = small[:, 0:1]
                lnv = small[:, 1:2]
                rstd = small[:, 2:3]
                act(a_bf[:], h[:, t, :], AF.Square, [f"h{t}"], ["a_bf", "ss"], accum=ss)
                act(lnv, ss, AF.Ln, ["ss"], ["lnv"], bias=EPS, scale=1.0 / D)
                act(rstd, lnv, AF.Exp, ["lnv"], ["rstd"], scale=-0.5)
                stt(outst[:, i, :], h[:, t, :], rstd, gvec[:], ALU.mult, ALU.mult, [f"h{t}", "rstd", "gvec"], [f"outst{i}"])
                dma("sp", y_d[t * 128:(t + 1) * 128, :], outst[:, i, :], [f"outst{i}"], [f"y{t}"], s_out[i])
            S.add("sp", None, r=[f"y{t}" for t in range(NT)], w=[])

        def dump_h():
            S.barrier()
            sd = S.dsem("dump")
            for t in range(NT):
                dma("sp", y_d[t * 128:(t + 1) * 128, :], h[:, t, :], [f"h{t}"], [f"y{t}"], sd)
            S.add("sp", None, r=[f"y{t}" for t in range(NT)], w=[])

        def program():
            load_x(xp_d if SI >= 1 else x_d)
            if SI == 0:
                return dump_h()
            mixer_main(0, None)
            save_state(0)
            if stop == 'mixA':
                return dump_h()
            S.barrier()
            ffn(0)
            if stop == 'ffnA':
                return dump_h()
            S.barrier()
            mixer_prepass(1)
            save_state(1)
            if stop == 'preA':
                return dump_h()
            S.barrier()
            load_x(x_d)
            for l in range(2):
                mixer_main(l, l)
                if stop == f'mix{l}':
                    return dump_h()
                S.barrier()
                ffn(l)
                if stop == f'ffn{l}':
                    return dump_h()
                S.barrier()
            final()

        program()
        print("ops:", {e: (len(S.ops[e]), sum(1 for o in S.ops[e] if o.mark)) for e in S.ENGS})
        S.emit(nc, stack)
    return nc


_CACHE = {}


def _prep_shared(inp):
    f = lambda a: np.ascontiguousarray(np.asarray(a, dtype=np.float32))
    consts = np.zeros((128, C_N), np.float32)
    consts[:, C_ID:C_ID + 128] = np.eye(128, dtype=np.float32)
    consts[:, C_ONES:C_ONES + 128] = 1.0 / 256.0
    p = np.arange(128)[:, None] % 64
    i = np.arange(64)[None, :]
    consts[:, C_TRI:C_TRI + 64] = (i >= p).astype(np.float32)
    sm = np.ones((128, 256), np.float32)
    sm[:, 0::64] = 0.0
    consts[:, C_SM:C_SM + 256] = sm
    pvec = np.zeros((2, 128, PV_N), np.float32)
    for l in range(2):
        scw = f(inp["sc_conv_w"][l])
        cfw = f(inp["cf_conv_w"][l])
        for cc in range(2):
            pvec[l, :, PV_SCW + cc * 3:PV_SCW + cc * 3 + 3] = scw[:, cc * 128:(cc + 1) * 128].T
            pvec[l, :, PV_CFW + cc * 31:PV_CFW + cc * 31 + 31] = cfw[:, cc * 128:(cc + 1) * 128].T
            pvec[l, :, PV_CFB + cc] = f(inp["cf_conv_b"][l])[cc * 128:(cc + 1) * 128]
            pvec[l, :, PV_LNG + cc] = f(inp["cf_ln_g"][l])[cc * 128:(cc + 1) * 128]
            pvec[l, :, PV_LNB + cc] = f(inp["cf_ln_b"][l])[cc * 128:(cc + 1) * 128]
            pvec[l, :, PV_BA + cc] = f(inp["gla_b_a"][l])[cc * 128:(cc + 1) * 128]
    bvec = np.zeros((5, 128, D), np.float32)
    bvec[0] = f(inp["attn_norm_g"][0])[None, :]
    bvec[1] = f(inp["attn_norm_g"][1])[None, :]
    bvec[2] = f(inp["ffn_norm_g"][0])[None, :]
    bvec[3] = f(inp["ffn_norm_g"][1])[None, :]
    bvec[4] = f(inp["final_norm_g"])[None, :]
    gng = np.zeros((2, 128, 512), np.float32)
    for l in range(2):
        gng[l] = f(inp["gla_norm_g"][l]).reshape(1, 512)
    shared = {
        "consts": consts, "pvec": pvec, "bvec": bvec, "gng": gng,
        "w_a2": f(inp["gla_w_a2"]), "w_in": f(inp["w_in"]), "w_out": f(inp["w_out"]),
        "dense_w_gate": f(inp["dense_w_gate"]), "dense_w_up": f(inp["dense_w_up"]), "dense_w_down": f(inp["dense_w_down"]),
        "moe_w_router": f(inp["moe_w_router"]), "moe_w_gate": f(inp["moe_w_gate"]), "moe_w_up": f(inp["moe_w_up"]),
        "moe_w_down": f(inp["moe_w_down"]),
    }
    return shared


def kernel(_stop=None, **inputs):
    if _stop is None:
        _stop = DBG_STOP
    x = np.asarray(inputs["x"], dtype=np.float32)
    shared = _prep_shared(inputs)
    if STAGES.index(_stop) < STAGES.index('ffn1'):
        for k_ in ("moe_w_gate", "moe_w_up", "moe_w_down"):
            shared.pop(k_)
    if _stop not in _CACHE:
        _CACHE[_stop] = build_program(_stop)
    nc = _CACHE[_stop]
    in_maps = []
    for r in range(NCORES):
        b, half = r // 2, r % 2
        sel = np.zeros((128, NCORES), np.float32)
        sel[:, 0] = float(half)
        m = dict(shared)
        m["x"] = np.ascontiguousarray(x[b, half * T:(half + 1) * T, :])
        m["x_prev"] = np.ascontiguousarray(x[b, 0:T, :]) if half == 1 else np.zeros((T, D), np.float32)
        m["sel"] = sel
        in_maps.append(m)
    res = run_bass_kernel_spmd(nc, in_maps, core_ids=list(range(NCORES)))
    out = np.zeros((4, 4096, D), np.float32)
    for r in range(NCORES):
        b, half = r // 2, r % 2
        out[b, half * T:(half + 1) * T, :] = np.asarray(res.results[r]["y"], dtype=np.float32)
    return out
```
